# Optimizing a Trainium2 kernel written in Bass

```python
import math
import jax, jax.numpy as jnp
from jax import lax
import numpy as np

D_MODEL = 2048
BATCH = 2
SEQ = 8192
DEPTH = 2

S5_WIDTH = D_MODEL // 2
S5_GROUP_CH = 16
S5_GROUPS = S5_WIDTH // S5_GROUP_CH
S5_STATE = 64
MLA_HEADS = 8
MLA_NOPE = 128
MLA_ROPE = 64
MLA_V = 128
MLA_Q_RANK = 512
MLA_KV_RANK = 512
ROPE_THETA = 10000.0
Q_BLOCK = 128
IN_COLS = S5_WIDTH + MLA_Q_RANK + MLA_KV_RANK + MLA_ROPE
MIX_OUT = S5_WIDTH + MLA_HEADS * MLA_V
CONV_WIDTH = D_MODEL
CONV_KERNEL = 31
N_EXPERTS = 32
TOP_K = 4
EXPERT_FF = D_MODEL
SWIGLU_ALPHA = 1.702
SWIGLU_LIMIT = 7.0
EXPERT_BLOCK = 128
EPS = 1e-6

kernel_name = 'hybrid_s5_mla_conformer_moe'


def rmsnorm(x, g):
    xf = x.astype(jnp.float32)
    y = xf * lax.rsqrt(jnp.mean(xf * xf, axis=-1, keepdims=True) + EPS)
    return (y * g.astype(jnp.float32)).astype(x.dtype)


def layernorm(x, g, b):
    xf = x.astype(jnp.float32)
    mu = jnp.mean(xf, axis=-1, keepdims=True)
    var = jnp.mean(jnp.square(xf - mu), axis=-1, keepdims=True)
    y = (xf - mu) * lax.rsqrt(var + 1e-5)
    return (y * g.astype(jnp.float32) + b.astype(jnp.float32)).astype(x.dtype)


def ada_modulation(c, w, b):
    m = jax.nn.silu(c) @ w + b
    shift, scale, gate = jnp.split(m, 3, axis=-1)
    return shift[:, None, :], scale[:, None, :], gate[:, None, :]


def rope_tables(positions):
    inv_freq = 1.0 / (ROPE_THETA ** (jnp.arange(0, MLA_ROPE, 2, dtype=jnp.float32) / MLA_ROPE))
    ang = positions.astype(jnp.float32)[..., None] * inv_freq
    return jnp.cos(ang), jnp.sin(ang)


def apply_rope(x, cos, sin):
    x1, x2 = jnp.split(x, 2, axis=-1)
    return jnp.concatenate([x1 * cos - x2 * sin, x1 * sin + x2 * cos], axis=-1).astype(x.dtype)


def _complex_scan_op(e1, e2):
    a1r, a1i, b1r, b1i = e1
    a2r, a2i, b2r, b2i = e2
    return (a2r * a1r - a2i * a1i,
            a2r * a1i + a2i * a1r,
            a2r * b1r - a2i * b1i + b2r,
            a2r * b1i + a2i * b1r + b2i)


def s5_mixer(u, lam_re, lam_im, log_step, b_re, b_im, c_re, c_im, d, w_glu, b_glu):
    bsz, seq, _ = u.shape
    f32 = jnp.float32
    uf = u.astype(f32).reshape(bsz, seq, S5_GROUPS, S5_GROUP_CH)
    lr = jnp.minimum(lam_re.astype(f32), -1e-4)
    li = lam_im.astype(f32)
    step = jnp.exp(log_step.astype(f32))[:, None]
    mag = jnp.exp(lr * step)
    ab_re = mag * jnp.cos(li * step)
    ab_im = mag * jnp.sin(li * step)
    denom = lr * lr + li * li
    nr, ni = ab_re - 1.0, ab_im
    ratio_re = (nr * lr + ni * li) / denom
    ratio_im = (ni * lr - nr * li) / denom
    br, bi = b_re.astype(f32), b_im.astype(f32)
    bb_re = ratio_re[..., None] * br - ratio_im[..., None] * bi
    bb_im = ratio_re[..., None] * bi + ratio_im[..., None] * br
    bu_re = jnp.einsum('blgh,gph->lbgp', uf, bb_re)
    bu_im = jnp.einsum('blgh,gph->lbgp', uf, bb_im)
    a_re = jnp.broadcast_to(ab_re[None, None], (seq, 1, S5_GROUPS, S5_STATE))
    a_im = jnp.broadcast_to(ab_im[None, None], (seq, 1, S5_GROUPS, S5_STATE))
    _, _, x_re, x_im = lax.associative_scan(_complex_scan_op, (a_re, a_im, bu_re, bu_im), axis=0)
    y = (jnp.einsum('lbgp,ghp->blgh', x_re, c_re.astype(f32))
         - jnp.einsum('lbgp,ghp->blgh', x_im, c_im.astype(f32))
         + d.astype(f32) * uf)
    y = jax.nn.gelu(y.reshape(bsz, seq, S5_WIDTH)).astype(u.dtype)
    return y * jax.nn.sigmoid(y @ w_glu + b_glu)


def causal_block_attention(q, k, v, scale):
    bsz, seq, nh, _ = q.shape
    dv = v.shape[-1]
    n_blocks = seq // Q_BLOCK
    key_pos = jnp.arange(seq)

    def one_block(i):
        qb = lax.dynamic_slice_in_dim(q, i * Q_BLOCK, Q_BLOCK, axis=1)
        s = jnp.einsum('bqhd,bkhd->bhqk', qb, k, preferred_element_type=jnp.float32) * scale
        q_pos = i * Q_BLOCK + jnp.arange(Q_BLOCK)
        s = jnp.where(key_pos[None, :] <= q_pos[:, None], s, -1e30)
        p = jax.nn.softmax(s, axis=-1).astype(v.dtype)
        return jnp.einsum('bhqk,bkhd->bqhd', p, v)

    o = lax.map(one_block, jnp.arange(n_blocks))
    return o.transpose(1, 0, 2, 3, 4).reshape(bsz, seq, nh * dv)


def mla_mixer(c_q, c_kv, k_rope, cos, sin, q_norm_g, w_uq, kv_norm_g, w_ukv):
    bsz, seq, _ = c_q.shape
    q = (rmsnorm(c_q, q_norm_g) @ w_uq).reshape(bsz, seq, MLA_HEADS, MLA_NOPE + MLA_ROPE)
    q_nope, q_pe = q[..., :MLA_NOPE], q[..., MLA_NOPE:]
    q_pe = apply_rope(q_pe, cos[:, :, None, :], sin[:, :, None, :])
    kv = (rmsnorm(c_kv, kv_norm_g) @ w_ukv).reshape(bsz, seq, MLA_HEADS, MLA_NOPE + MLA_V)
    k_nope, v = kv[..., :MLA_NOPE], kv[..., MLA_NOPE:]
    k_pe = apply_rope(k_rope, cos, sin)[:, :, None, :]
    k_pe = jnp.broadcast_to(k_pe, (bsz, seq, MLA_HEADS, MLA_ROPE))
    q = jnp.concatenate([q_nope, q_pe], axis=-1)
    k = jnp.concatenate([k_nope, k_pe], axis=-1)
    return causal_block_attention(q, k, v, 1.0 / math.sqrt(MLA_NOPE + MLA_ROPE))


def s5_mla_sublayer(x, c, cos, sin, norm_g, ada_w, ada_b, w_in,
                    lam_re, lam_im, log_step, b_re, b_im, c_re, c_im, d, w_glu, b_glu,
                    q_norm_g, w_uq, kv_norm_g, w_ukv, w_out):
    shift, scale, gate = ada_modulation(c, ada_w, ada_b)
    h = rmsnorm(x, norm_g) * (1.0 + scale) + shift
    proj = h @ w_in
    cut = [S5_WIDTH, S5_WIDTH + MLA_Q_RANK, S5_WIDTH + MLA_Q_RANK + MLA_KV_RANK]
    u, c_q, c_kv, k_rope = jnp.split(proj, cut, axis=-1)
    y_s5 = s5_mixer(u, lam_re, lam_im, log_step, b_re, b_im, c_re, c_im, d, w_glu, b_glu)
    y_mla = mla_mixer(c_q, c_kv, k_rope, cos, sin, q_norm_g, w_uq, kv_norm_g, w_ukv)
    y = jnp.concatenate([y_s5, y_mla], axis=-1) @ w_out
    return x + gate * y


def conv_sublayer(x, c, norm_g, ada_w, ada_b, w_pw1, b_pw1, w_dw, b_dw, ln_g, ln_b, w_pw2, b_pw2):
    shift, scale, gate = ada_modulation(c, ada_w, ada_b)
    h = rmsnorm(x, norm_g) * (1.0 + scale) + shift
    y = jax.nn.glu(h @ w_pw1 + b_pw1, axis=-1)
    y = lax.conv_general_dilated(
        y, w_dw[:, None, :].astype(y.dtype), window_strides=(1,),
        padding=[(CONV_KERNEL - 1, 0)], dimension_numbers=('NWC', 'WIO', 'NWC'),
        feature_group_count=CONV_WIDTH) + b_dw
    y = jax.nn.silu(layernorm(y, ln_g, ln_b))
    return x + gate * (y @ w_pw2 + b_pw2)


def moe_ffn(h, router_w, router_b, w1, b1, w2, b2):
    bsz, seq, dm = h.shape
    xf = h.reshape(-1, dm)
    n_tok = xf.shape[0]
    logits = (xf @ router_w + router_b).astype(jnp.float32)
    top_v, top_i = lax.top_k(logits, TOP_K)
    gates = jax.nn.softmax(top_v, axis=-1).astype(h.dtype)
    n_assign = n_tok * TOP_K
    flat_e = top_i.reshape(-1).astype(jnp.int32)
    flat_tok = jnp.arange(n_assign, dtype=jnp.int32) // TOP_K
    flat_g = gates.reshape(-1)
    order = jnp.argsort(flat_e)
    sorted_e = flat_e[order]
    counts = jnp.bincount(flat_e, length=N_EXPERTS)
    padded = ((counts + EXPERT_BLOCK - 1) // EXPERT_BLOCK) * EXPERT_BLOCK
    pad_end = jnp.cumsum(padded)
    pad_start = pad_end - padded
    start = jnp.cumsum(counts) - counts
    rank = jnp.arange(n_assign, dtype=jnp.int32) - start[sorted_e]
    dest = pad_start[sorted_e] + rank
    n_blocks = (n_assign + EXPERT_BLOCK - 1) // EXPERT_BLOCK + N_EXPERTS
    n_slots = n_blocks * EXPERT_BLOCK
    slot_tok = jnp.zeros((n_slots,), jnp.int32).at[dest].set(flat_tok[order])
    slot_w = jnp.zeros((n_slots,), h.dtype).at[dest].set(flat_g[order])
    block_start = jnp.arange(n_blocks, dtype=jnp.int32) * EXPERT_BLOCK
    block_expert = jnp.clip(jnp.searchsorted(pad_end, block_start, side='right'), 0, N_EXPERTS - 1)

    def expert_block(args):
        tok, e = args
        a = xf[tok] @ w1[e] + b1[e]
        a_glu = jnp.minimum(a[:, ::2], SWIGLU_LIMIT)
        a_lin = jnp.clip(a[:, 1::2], -SWIGLU_LIMIT, SWIGLU_LIMIT)
        act = a_glu * jax.nn.sigmoid(SWIGLU_ALPHA * a_glu) * (a_lin + 1.0)
        return act @ w2[e] + b2[e]

    y = lax.map(expert_block, (slot_tok.reshape(n_blocks, EXPERT_BLOCK), block_expert))
    y = y.reshape(n_slots, dm) * slot_w[:, None]
    out = jnp.zeros_like(xf).at[slot_tok].add(y)
    return out.reshape(bsz, seq, dm)


def moe_sublayer(x, c, norm_g, ada_w, ada_b, router_w, router_b, w1, b1, w2, b2):
    shift, scale, gate = ada_modulation(c, ada_w, ada_b)
    h = rmsnorm(x, norm_g) * (1.0 + scale) + shift
    return x + gate * moe_ffn(h, router_w, router_b, w1, b1, w2, b2)


def setup_inputs(seed: int = 0) -> dict:
    key = jax.random.key(seed)
    it = iter(list(jax.random.split(key, 80)))
    f32 = jnp.float32

    def nrm(shape, scale):
        return jax.random.normal(next(it), shape, f32) * scale

    def gain(n):
        return 1.0 + nrm((n,), 0.02)

    def ada():
        return nrm((D_MODEL, 3 * D_MODEL), 0.5 * D_MODEL ** -0.5), nrm((3 * D_MODEL,), 0.02)

    def moe_params(prefix, p):
        p[prefix + 'moe_norm_g'] = gain(D_MODEL)
        p[prefix + 'moe_ada_w'], p[prefix + 'moe_ada_b'] = ada()
        p[prefix + 'router_w'] = nrm((D_MODEL, N_EXPERTS), D_MODEL ** -0.5)
        p[prefix + 'router_b'] = nrm((N_EXPERTS,), 0.01)
        p[prefix + 'exp_w1'] = nrm((N_EXPERTS, D_MODEL, 2 * EXPERT_FF), D_MODEL ** -0.5)
        p[prefix + 'exp_b1'] = nrm((N_EXPERTS, 2 * EXPERT_FF), 0.01)
        p[prefix + 'exp_w2'] = nrm((N_EXPERTS, EXPERT_FF, D_MODEL), EXPERT_FF ** -0.5)
        p[prefix + 'exp_b2'] = nrm((N_EXPERTS, D_MODEL), 0.01)

    p = {}
    p['x'] = nrm((BATCH, SEQ, D_MODEL), 1.0)
    p['c'] = nrm((BATCH, D_MODEL), 1.0)
    p['positions'] = (jnp.arange(SEQ, dtype=jnp.int32)[None, :]
                      + jax.random.randint(next(it), (BATCH, 1), 0, 4096, dtype=jnp.int32))
    p['l0_mix_norm_g'] = gain(D_MODEL)
    p['l0_mix_ada_w'], p['l0_mix_ada_b'] = ada()
    p['l0_w_in'] = nrm((D_MODEL, IN_COLS), D_MODEL ** -0.5)
    p['l0_s5_lambda_re'] = -0.5 + nrm((S5_GROUPS, S5_STATE), 0.01)
    p['l0_s5_lambda_im'] = jnp.tile(math.pi * jnp.arange(S5_STATE, dtype=f32)[None, :], (S5_GROUPS, 1))
    p['l0_s5_log_step'] = jax.random.uniform(next(it), (S5_GROUPS,), f32, math.log(0.001), math.log(0.1))
    p['l0_s5_b_re'] = nrm((S5_GROUPS, S5_STATE, S5_GROUP_CH), (2.0 * S5_GROUP_CH) ** -0.5)
    p['l0_s5_b_im'] = nrm((S5_GROUPS, S5_STATE, S5_GROUP_CH), (2.0 * S5_GROUP_CH) ** -0.5)
    p['l0_s5_c_re'] = nrm((S5_GROUPS, S5_GROUP_CH, S5_STATE), (2.0 * S5_STATE) ** -0.5)
    p['l0_s5_c_im'] = nrm((S5_GROUPS, S5_GROUP_CH, S5_STATE), (2.0 * S5_STATE) ** -0.5)
    p['l0_s5_d'] = nrm((S5_GROUPS, S5_GROUP_CH), 1.0)
    p['l0_s5_w_glu'] = nrm((S5_WIDTH, S5_WIDTH), S5_WIDTH ** -0.5)
    p['l0_s5_b_glu'] = nrm((S5_WIDTH,), 0.01)
    p['l0_mla_q_norm_g'] = gain(MLA_Q_RANK)
    p['l0_mla_w_uq'] = nrm((MLA_Q_RANK, MLA_HEADS * (MLA_NOPE + MLA_ROPE)), MLA_Q_RANK ** -0.5)
    p['l0_mla_kv_norm_g'] = gain(MLA_KV_RANK)
    p['l0_mla_w_ukv'] = nrm((MLA_KV_RANK, MLA_HEADS * (MLA_NOPE + MLA_V)), MLA_KV_RANK ** -0.5)
    p['l0_w_out'] = nrm((MIX_OUT, D_MODEL), MIX_OUT ** -0.5)
    moe_params('l0_', p)
    p['l1_mix_norm_g'] = gain(D_MODEL)
    p['l1_mix_ada_w'], p['l1_mix_ada_b'] = ada()
    p['l1_conv_w_pw1'] = nrm((D_MODEL, 2 * CONV_WIDTH), D_MODEL ** -0.5)
    p['l1_conv_b_pw1'] = nrm((2 * CONV_WIDTH,), 0.01)
    p['l1_conv_w_dw'] = nrm((CONV_KERNEL, CONV_WIDTH), CONV_KERNEL ** -0.5)
    p['l1_conv_b_dw'] = nrm((CONV_WIDTH,), 0.01)
    p['l1_conv_ln_g'] = gain(CONV_WIDTH)
    p['l1_conv_ln_b'] = nrm((CONV_WIDTH,), 0.01)
    p['l1_conv_w_pw2'] = nrm((CONV_WIDTH, D_MODEL), CONV_WIDTH ** -0.5)
    p['l1_conv_b_pw2'] = nrm((D_MODEL,), 0.01)
    moe_params('l1_', p)
    p['final_norm_g'] = gain(D_MODEL)
    return p


def reference(x, c, positions,
              l0_mix_norm_g, l0_mix_ada_w, l0_mix_ada_b, l0_w_in,
              l0_s5_lambda_re, l0_s5_lambda_im, l0_s5_log_step, l0_s5_b_re, l0_s5_b_im,
              l0_s5_c_re, l0_s5_c_im, l0_s5_d, l0_s5_w_glu, l0_s5_b_glu,
              l0_mla_q_norm_g, l0_mla_w_uq, l0_mla_kv_norm_g, l0_mla_w_ukv, l0_w_out,
              l0_moe_norm_g, l0_moe_ada_w, l0_moe_ada_b, l0_router_w, l0_router_b,
              l0_exp_w1, l0_exp_b1, l0_exp_w2, l0_exp_b2,
              l1_mix_norm_g, l1_mix_ada_w, l1_mix_ada_b, l1_conv_w_pw1, l1_conv_b_pw1,
              l1_conv_w_dw, l1_conv_b_dw, l1_conv_ln_g, l1_conv_ln_b, l1_conv_w_pw2, l1_conv_b_pw2,
              l1_moe_norm_g, l1_moe_ada_w, l1_moe_ada_b, l1_router_w, l1_router_b,
              l1_exp_w1, l1_exp_b1, l1_exp_w2, l1_exp_b2,
              final_norm_g):
    cos, sin = rope_tables(positions)
    mixer_params = (
        (l0_mix_norm_g, l0_mix_ada_w, l0_mix_ada_b, l0_w_in,
         l0_s5_lambda_re, l0_s5_lambda_im, l0_s5_log_step, l0_s5_b_re, l0_s5_b_im,
         l0_s5_c_re, l0_s5_c_im, l0_s5_d, l0_s5_w_glu, l0_s5_b_glu,
         l0_mla_q_norm_g, l0_mla_w_uq, l0_mla_kv_norm_g, l0_mla_w_ukv, l0_w_out),
        (l1_mix_norm_g, l1_mix_ada_w, l1_mix_ada_b, l1_conv_w_pw1, l1_conv_b_pw1,
         l1_conv_w_dw, l1_conv_b_dw, l1_conv_ln_g, l1_conv_ln_b, l1_conv_w_pw2, l1_conv_b_pw2),
    )
    moe_params = (
        (l0_moe_norm_g, l0_moe_ada_w, l0_moe_ada_b, l0_router_w, l0_router_b,
         l0_exp_w1, l0_exp_b1, l0_exp_w2, l0_exp_b2),
        (l1_moe_norm_g, l1_moe_ada_w, l1_moe_ada_b, l1_router_w, l1_router_b,
         l1_exp_w1, l1_exp_b1, l1_exp_w2, l1_exp_b2),
    )
    for layer in range(DEPTH):
        if layer % 2 == 0:
            x = s5_mla_sublayer(x, c, cos, sin, *mixer_params[layer])
        else:
            x = conv_sublayer(x, c, *mixer_params[layer])
        x = moe_sublayer(x, c, *moe_params[layer])
    return rmsnorm(x, final_norm_g)
```

```python
import contextlib
import math
import numpy as np
import concourse.bass as bass
import concourse.mybir as mybir
from concourse.bass_utils import run_bass_kernel_spmd

F32 = mybir.dt.float32
BF16 = mybir.dt.bfloat16
I32 = mybir.dt.int32
AF = mybir.ActivationFunctionType
ALU = mybir.AluOpType
AX = mybir.AxisListType

D = 2048
KT = 16
TB = 512
NE_FULL = 32


class Tok:
    __slots__ = ("w", "r")

    def __init__(self):
        self.w = []
        self.r = []


class EngState:
    def __init__(self, name, h, sem):
        self.name = name
        self.h = h
        self.sem = sem
        self.n = 0
        self.waited = {}
        self.dsems = []
        self.dnext = 0


class Sched:
    def __init__(self, nc, ndma_sems=8):
        self.nc = nc
        self.es = contextlib.ExitStack()
        self.sems = {}
        self.E = {}
        for name, h in (("pe", nc.tensor), ("act", nc.scalar), ("dve", nc.vector),
                        ("pool", nc.gpsimd), ("sp", nc.sync)):
            sem = self.es.enter_context(nc.semaphore("s_" + name))
            self.sems[id(sem)] = sem
            self.E[name] = EngState(name, h, sem)
        for qn in ("sp", "act", "pool"):
            e = self.E[qn]
            for i in range(ndma_sems):
                s = self.es.enter_context(nc.semaphore("d_%s%d" % (qn, i)))
                self.sems[id(s)] = s
                e.dsems.append([s, 0])
        self.same = {"pe": False, "act": True, "dve": True, "pool": True, "sp": True}
        self.nops = 0
        self.psn = 0
        self.pst = []
        self.scopes = []
        self.nrot = 8

    def sbuf(self, name, shape, dtype):
        st = self.scopes[-1] if self.scopes else self.es
        return st.enter_context(self.nc.sbuf_tensor(name, list(shape), dtype))

    def barrier(self):
        evs = []
        for e in self.E.values():
            if e.n > 0:
                evs.append((id(e.sem), e.n))
            for s_, v in e.dsems:
                if v > 0:
                    evs.append((id(s_), v))
        for e in self.E.values():
            self._wait(e, [ev for ev in evs if ev[0] != id(e.sem)])

    @contextlib.contextmanager
    def scope(self):
        st = contextlib.ExitStack()
        self.scopes.append(st)
        try:
            yield
        finally:
            self.barrier()
            self.scopes.pop()
            st.close()

    def psum_init(self):
        for i in range(8):
            t = self.es.enter_context(self.nc.psum_tensor("ps%d" % i, [128, 512], F32))
            self.pst.append((t, Tok()))

    def ps(self):
        r = self.pst[self.psn % self.nrot]
        self.psn += 1
        return r

    def _wait(self, e, deps):
        best = {}
        for (sk, v) in deps:
            if v > best.get(sk, 0):
                best[sk] = v
        for sk, v in best.items():
            if e.waited.get(sk, 0) >= v:
                continue
            if sk == id(e.sem) and not self.same[e.name]:
                continue
            e.h.wait_ge(self.sems[sk], v)
            e.waited[sk] = v

    def _deps(self, r, w):
        deps = []
        for t in r:
            deps.extend(t.w)
        for t in w:
            deps.extend(t.w)
            deps.extend(t.r)
        return deps

    def _commit(self, ev, r, w):
        for t in r:
            t.r.append(ev)
            if len(t.r) > 48:
                best = {}
                for (sk, v) in t.r:
                    if v > best.get(sk, 0):
                        best[sk] = v
                t.r = list(best.items())
        for t in w:
            t.w = [ev]
            t.r = []

    def op(self, eng, fn, r=(), w=()):
        e = self.E[eng]
        self._wait(e, self._deps(r, w))
        ins = fn(e.h)
        e.n += 1
        ins.then_inc(e.sem, 1)
        self._commit((id(e.sem), e.n), r, w)
        self.nops += 1
        return ins

    def dma(self, q, out, in_, r=(), w=(), **kw):
        e = self.E[q]
        slot = e.dsems[e.dnext % len(e.dsems)]
        e.dnext += 1
        deps = self._deps(r, w)
        if slot[1] > 0:
            deps.append((id(slot[0]), slot[1]))
        self._wait(e, deps)
        ins = e.h.dma_start(out=out, in_=in_, **kw)
        slot[1] += 16
        ins.then_inc(slot[0], 16)
        self._commit((id(slot[0]), slot[1]), r, w)
        self.nops += 1
        return ins

    def finish(self, toks, eng="sp"):
        e = self.E[eng]
        deps = []
        for t in toks:
            deps.extend(t.w)
        self._wait(e, deps)


class Ring:
    def __init__(self, S, name, shape, dtype, n):
        self.b = [(S.sbuf("%s%d" % (name, i), shape, dtype), Tok()) for i in range(n)]
        self.i = 0

    def next(self):
        r = self.b[self.i % len(self.b)]
        self.i += 1
        return r


def build(cfg):
    B, L, NE = cfg["B"], cfg["L"], cfg["NE"]
    dbg = cfg.get("dbg")
    NB = L // TB
    NT = L // 128
    nc = bass.Bass("TRN2", target_bir_lowering=False)
    S = Sched(nc)
    S.psum_init()
    S.es.enter_context(nc.allow_non_contiguous_dma(reason="small strided parameter loads"))

    def din(name, shape, dt=F32):
        return nc.dram_tensor(name, list(shape), dt, kind="ExternalInput").ap()

    I = {}
    I["x"] = din("x", [B * L, D])
    I["c"] = din("c", [B, D])
    I["positions"] = din("positions", [B, L], I32)
    for pfx in ("l0_mix", "l0_moe", "l1_mix", "l1_moe"):
        I[pfx + "_norm_g"] = din(pfx + "_norm_g", [D])
        I[pfx + "_ada_w"] = din(pfx + "_ada_w", [D, 3 * D])
        I[pfx + "_ada_b"] = din(pfx + "_ada_b", [3 * D])
    I["l0_w_in"] = din("l0_w_in", [D, 2112])
    I["l0_s5_lambda_re"] = din("l0_s5_lambda_re", [64, 64])
    I["l0_s5_lambda_im"] = din("l0_s5_lambda_im", [64, 64])
    I["l0_s5_log_step"] = din("l0_s5_log_step", [64])
    I["l0_s5_b_re"] = din("l0_s5_b_re", [64, 64, 16])
    I["l0_s5_b_im"] = din("l0_s5_b_im", [64, 64, 16])
    I["l0_s5_c_re"] = din("l0_s5_c_re", [64, 16, 64])
    I["l0_s5_c_im"] = din("l0_s5_c_im", [64, 16, 64])
    I["l0_s5_d"] = din("l0_s5_d", [64, 16])
    I["l0_s5_w_glu"] = din("l0_s5_w_glu", [1024, 1024])
    I["l0_s5_b_glu"] = din("l0_s5_b_glu", [1024])
    I["l0_mla_q_norm_g"] = din("l0_mla_q_norm_g", [512])
    I["l0_mla_w_uq"] = din("l0_mla_w_uq", [512, 1536])
    I["l0_mla_kv_norm_g"] = din("l0_mla_kv_norm_g", [512])
    I["l0_mla_w_ukv"] = din("l0_mla_w_ukv", [512, 2048])
    I["l0_w_out"] = din("l0_w_out", [D, D])
    for l in ("l0_", "l1_"):
        I[l + "router_w"] = din(l + "router_w", [D, NE])
        I[l + "router_b"] = din(l + "router_b", [NE])
        I[l + "exp_w1"] = din(l + "exp_w1", [NE * D, 2 * D])
        I[l + "exp_b1"] = din(l + "exp_b1", [NE, 2 * D])
        I[l + "exp_w2"] = din(l + "exp_w2", [NE * D, D])
        I[l + "exp_b2"] = din(l + "exp_b2", [NE, D])
    I["l1_conv_w_pw1"] = din("l1_conv_w_pw1", [D, 2 * D])
    I["l1_conv_b_pw1"] = din("l1_conv_b_pw1", [2 * D])
    I["l1_conv_w_dw"] = din("l1_conv_w_dw", [31, D])
    I["l1_conv_b_dw"] = din("l1_conv_b_dw", [D])
    I["l1_conv_ln_g"] = din("l1_conv_ln_g", [D])
    I["l1_conv_ln_b"] = din("l1_conv_ln_b", [D])
    I["l1_conv_w_pw2"] = din("l1_conv_w_pw2", [D, D])
    I["l1_conv_b_pw2"] = din("l1_conv_b_pw2", [D])
    I["final_norm_g"] = din("final_norm_g", [D])
    split = cfg.get("nown")
    out = nc.dram_tensor("out", [split * TB if split else B * L, D], F32, kind="ExternalOutput").ap()
    tout = Tok()
    if split:
        I["selF"] = din("selF", [128, (split + 1) * (L // TB)], F32)
        I["hv"] = din("hv", [128, 1], F32)

    xres = [nc.dram_tensor("xres%d" % b, [128, KT, L], F32).ap() for b in range(B)]
    txres = [[Tok() for _ in range(NB)] for b in range(B)]

    ident = S.sbuf("ident", [128, 128], F32)
    ones = S.sbuf("ones", [128, 128], F32)
    tc_ = Tok()
    S.op("pool", lambda e: e.memset(ones[:], 1.0), w=[tc_])
    S.op("pool", lambda e: e.affine_select(out=ident[:], in_=ones[:], pattern=[[1, 128]],
                                           compare_op=ALU.is_equal, fill=0.0, base=0,
                                           channel_multiplier=-1), r=[tc_], w=[tc_])

    def col_load(name, src, n, q="sp"):
        t = S.sbuf(name, [128, n], F32)
        tk = Tok()
        S.dma(q, t[:], src.rearrange("(t p) -> p t", p=128), w=[tk], allow_slow_non_contiguous=True) \
            if False else S.dma(q, t[:], src.rearrange("(t p) -> p t", p=128), w=[tk])
        return t, tk

    wring = Ring(S, "wch", [128, KT, 128], BF16, 3)

    def lin_group(hT, thT, kt_n, W, col0, ncols, ncol_tok, rhs_slice=None, lhs_step=1):
        wt, wtk = wring.next()
        src = W.rearrange("(kt p) c -> p kt c", p=128)[:, :, col0:col0 + ncols * lhs_step]
        S.dma("pool", wt[:, 0:kt_n, 0:ncols * lhs_step], src, w=[wtk])
        ps, ptk = S.ps()
        for kt in range(kt_n):
            lhs = wt[:, kt, 0:ncols * lhs_step:lhs_step] if lhs_step > 1 else wt[:, kt, 0:ncols]
            rhs = hT[:, kt, 0:ncol_tok] if rhs_slice is None else rhs_slice(kt)
            S.op("pe", lambda e, lhs=lhs, rhs=rhs, kt=kt: e.matmul(
                ps[0:ncols, 0:ncol_tok], lhsT=lhs, rhs=rhs, start=(kt == 0), stop=(kt == kt_n - 1)),
                r=[wtk, thT], w=[ptk])
        return ps, ptk

    sc = S.sbuf("silu_c", [128, KT, 2], F32)
    tsc = Tok()
    S.op("dve", lambda e: e.memset(sc[:], 0.0), w=[tsc])
    for b in range(B):
        S.dma("sp", sc[:, :, b], I["c"][b].rearrange("(t p) -> p t", p=128), w=[tsc])
    S.op("act", lambda e: e.activation(out=sc[:], in_=sc[:], func=AF.Silu), r=[tsc], w=[tsc])
    mods = {}
    premod = {pfx: (S.sbuf(pfx + "_mod", [128, 48, 2], F32), S.sbuf(pfx + "_A", [128, KT, 2], F32))
              for pfx in ("l0_mix", "l0_moe", "l1_mix", "l1_moe")}
    ada_scope = S.scope()
    ada_scope.__enter__()
    adaring = Ring(S, "adaw", [128, KT, 128], F32, 2)
    for pfx in ("l0_mix", "l0_moe", "l1_mix", "l1_moe"):
        ab, tab = col_load(pfx + "_adab", I[pfx + "_ada_b"], 48)
        ng, tng = col_load(pfx + "_ng", I[pfx + "_norm_g"], 16)
        md, Am = premod[pfx]
        tmd = Tok()
        Wv = I[pfx + "_ada_w"].rearrange("(kt p) c -> p kt c", p=128)
        for m in range(48):
            wt, wtk = adaring.next()
            S.dma("sp" if m % 2 == 0 else "act", wt[:], Wv[:, :, m * 128:(m + 1) * 128], w=[wtk])
            ps, ptk = S.ps()
            for kt in range(KT):
                S.op("pe", lambda e, kt=kt: e.matmul(ps[:, 0:2], lhsT=wt[:, kt, :], rhs=sc[:, kt, :],
                                                     start=(kt == 0), stop=(kt == KT - 1)),
                     r=[wtk, tsc], w=[ptk])
            S.op("dve", lambda e, m=m: e.tensor_scalar(out=md[:, m, :], in0=ps[:, 0:2], scalar1=ab[:, m:m + 1],
                                                       scalar2=None, op0=ALU.add), r=[ptk, tab], w=[tmd])
        S.op("dve", lambda e: e.tensor_scalar(out=Am[:], in0=md[:, 16:32, :], scalar1=1.0, scalar2=None,
                                              op0=ALU.add), r=[tmd], w=[tmd])
        for b in range(2):
            S.op("dve", lambda e, b=b: e.tensor_tensor(out=Am[:, :, b], in0=Am[:, :, b], in1=ng[:], op=ALU.mult),
                 r=[tmd, tng], w=[tmd])
        mods[pfx] = (md, Am, tmd)
    ada_scope.__exit__(None, None, None)

    xT = S.sbuf("xT", [128, KT, TB], F32)
    txT = Tok()
    hT = S.sbuf("hT", [128, KT, TB], BF16)
    thT = Tok()
    sqring = Ring(S, "sq", [128, TB], F32, 2)
    rstd = S.sbuf("rstd", [128, TB], F32)
    trstd = Tok()
    tmpring = Ring(S, "tmpf", [128, TB], F32, 3)

    def front(pfx, b):
        md, Am, tmd = mods[pfx]
        ps, ptk = S.ps()
        for kt in range(KT):
            sq, tsq = sqring.next()
            S.op("act", lambda e, kt=kt: e.activation(out=sq[:], in_=xT[:, kt, :], func=AF.Square), r=[txT], w=[tsq])
            S.op("pe", lambda e, kt=kt: e.matmul(ps[:], lhsT=ones[:], rhs=sq[:], start=(kt == 0), stop=(kt == KT - 1)),
                 r=[tsq, tc_], w=[ptk])
        S.op("dve", lambda e: e.tensor_scalar(out=rstd[:], in0=ps[:], scalar1=1.0 / D, scalar2=1e-6,
                                              op0=ALU.mult, op1=ALU.add), r=[ptk], w=[trstd])
        S.op("act", lambda e: e.activation(out=rstd[:], in_=rstd[:], func=AF.Sqrt), r=[trstd], w=[trstd])
        S.op("dve", lambda e: e.reciprocal(out=rstd[:], in_=rstd[:]), r=[trstd], w=[trstd])
        for kt in range(KT):
            tm, ttm = tmpring.next()
            S.op("dve", lambda e, kt=kt: e.tensor_tensor(out=tm[:], in0=xT[:, kt, :], in1=rstd[:], op=ALU.mult),
                 r=[txT, trstd], w=[ttm])
            S.op("pool", lambda e, kt=kt: e.tensor_scalar(out=hT[:, kt, :], in0=tm[:], scalar1=Am[:, kt, b:b + 1],
                                                          scalar2=md[:, kt, b:b + 1], op0=ALU.mult, op1=ALU.add),
                 r=[ttm, tmd], w=[thT])

    def load_x_block(b, blk):
        S.dma("sp", xT[:], xres[b][:, :, blk * TB:(blk + 1) * TB], r=[txres[b][blk]], w=[txT])

    def store_x_block(b, blk):
        S.dma("sp", xres[b][:, :, blk * TB:(blk + 1) * TB], xT[:], r=[txT], w=[txres[b][blk]])

    if split:
        selF = S.sbuf("selF_sb", [128, (split + 1) * NB], F32)
        tself = Tok()
        S.dma("sp", selF[:], I["selF"], w=[tself])
        hv_sb = S.sbuf("hv_sb", [128, 1], F32)
        thv = Tok()
        S.dma("sp", hv_sb[:], I["hv"], w=[thv])
        XL = nc.dram_tensor("xresL", [128, KT, (split + 1) * TB], F32).ap()
        tXL = [Tok() for _ in range(split + 1)]

    def load_sel(i):
        allx = [t for row in txres for t in row]
        for kt in range(KT):
            for blk in range(NB):
                tm, ttm = tmpring.next()
                S.dma("sp" if (kt + blk) % 2 == 0 else "act", tm[:], xres[0][:, kt, blk * TB:(blk + 1) * TB], r=allx, w=[ttm])
                col = selF[:, i * NB + blk:i * NB + blk + 1]
                if blk == 0:
                    S.op("dve", lambda e, kt=kt: e.tensor_scalar(out=xT[:, kt, :], in0=tm[:], scalar1=col, scalar2=None,
                                                                 op0=ALU.mult), r=[ttm, tself], w=[txT])
                else:
                    S.op("dve", lambda e, kt=kt: e.scalar_tensor_tensor(out=xT[:, kt, :], in0=tm[:], scalar=col, in1=xT[:, kt, :],
                                                                        op0=ALU.mult, op1=ALU.add), r=[ttm, tself, txT], w=[txT])

    def load_loc(i):
        S.dma("sp", xT[:], XL[:, :, i * TB:(i + 1) * TB], r=[tXL[i]], w=[txT])

    def store_loc(i):
        S.dma("sp", XL[:, :, i * TB:(i + 1) * TB], xT[:], r=[txT], w=[tXL[i]])

    def blks(b, phase):
        if not split:
            return [((lambda blk=blk: load_x_block(b, blk)), (lambda blk=blk: store_x_block(b, blk)),
                     {"halo": False, "orow": b * L + blk * TB}) for blk in range(NB)]
        lst = []
        for i in (range(0, split + 1) if phase in ("moe0", "conv") else range(1, split + 1)):
            ld = (lambda i=i: load_sel(i)) if phase == "moe0" else (lambda i=i: load_loc(i))
            lst.append((ld, (lambda i=i: store_loc(i)), {"halo": i == 0, "orow": (i - 1) * TB}))
        return lst

    def stage0():
      xin_ring = Ring(S, "xin", [128, D], F32, 2)
      for b in range(B):
        for blk in range(NB):
            for s in range(4):
                xi, txi = xin_ring.next()
                r0 = b * L + blk * TB + s * 128
                S.dma("sp" if s % 2 == 0 else "act", xi[:], I["x"][r0:r0 + 128, :], w=[txi])
                for g in range(4):
                    ps, ptk = S.ps()
                    for j in range(4):
                        kt = g * 4 + j
                        S.op("pe", lambda e, kt=kt, j=j: e.transpose(ps[:, j * 128:(j + 1) * 128],
                                                                     xi[:, kt * 128:(kt + 1) * 128], ident[:]),
                             r=[txi, tc_], w=[ptk])
                    S.op("act" if g % 2 else "dve", lambda e, g=g, s=s: (e.activation(
                        out=xT[:, g * 4:(g + 1) * 4, s * 128:(s + 1) * 128],
                        in_=ps[:].rearrange("p (j t) -> p j t", j=4), func=AF.Copy) if g % 2 else e.tensor_copy(
                        xT[:, g * 4:(g + 1) * 4, s * 128:(s + 1) * 128], ps[:].rearrange("p (j t) -> p j t", j=4))),
                        r=[ptk], w=[txT])
            store_x_block(b, blk)

    def moe(layer):
        pfx = "l%d_moe" % layer
        lp = "l%d_" % layer
        md, Am, tmd = mods[pfx]
        rw = S.sbuf(lp + "rw", [128, KT, NE], BF16)
        trw = Tok()
        S.dma("pool", rw[:], I[lp + "router_w"].rearrange("(kt p) e -> p kt e", p=128), w=[trw])
        rb = S.sbuf(lp + "rb", [128, NE], F32)
        trb = Tok()
        S.dma("sp", rb[:], I[lp + "router_b"].rearrange("(o e) -> o e", o=1).partition_broadcast(128)
              if False else I[lp + "router_b"].partition_broadcast(128), w=[trb])
        b1n = S.sbuf(lp + "b1n", [NE, 2 * D], F32)
        b2n = S.sbuf(lp + "b2n", [NE, D], F32)
        tbn = Tok()
        S.dma("sp", b1n[:], I[lp + "exp_b1"], w=[tbn])
        S.dma("sp", b2n[:], I[lp + "exp_b2"], w=[tbn])
        b1g = S.sbuf(lp + "b1g", [128, 16, NE], F32)
        b1l = S.sbuf(lp + "b1l", [128, 16, NE], F32)
        b2c = S.sbuf(lp + "b2c", [128, 16, NE], F32)
        tb1 = Tok()
        for j in range(16):
            for (dst, srcap) in ((b1g, b1n[:, j * 256:(j + 1) * 256:2]), (b1l, b1n[:, j * 256 + 1:(j + 1) * 256:2]),
                                 (b2c, b2n[:, j * 128:(j + 1) * 128])):
                ps, ptk = S.ps()
                S.op("pe", lambda e, srcap=srcap: e.transpose(ps[:, 0:NE], srcap, ident[0:NE, 0:NE]),
                     r=[tbn, tc_], w=[ptk])
                S.op("dve", lambda e, dst=dst, j=j: e.tensor_copy(dst[:, j, :], ps[:, 0:NE]), r=[ptk], w=[tb1])
        sel = S.sbuf(lp + "sel", [NE, NE, 128], F32)
        tsel = Tok()
        S.op("dve", lambda e: e.tensor_copy(sel[:], ident[0:NE, 0:NE].unsqueeze(2).to_broadcast([NE, NE, 128])),
             r=[tc_], w=[tsel])
        gT = S.sbuf(lp + "gT", [NE, TB], F32)
        tgT = Tok()
        actT = S.sbuf(lp + "actT", [128, 16, TB], BF16)
        tact = [Tok() for _ in range(16)]
        gbc = S.sbuf(lp + "gbc", [128, TB], F32)
        tgbc = Tok()
        w1ring = Ring(S, lp + "w1c", [128, KT, 512], BF16, 2)
        w2ring = Ring(S, lp + "w2c", [128, KT, 128], BF16, 2)
        lg = S.sbuf(lp + "lg", [128, NE], F32)
        m8 = S.sbuf(lp + "m8", [128, 8], F32)
        ex = S.sbuf(lp + "ex", [128, NE], F32)
        msk = S.sbuf(lp + "msk", [128, NE], F32)
        den = S.sbuf(lp + "den", [128, 1], F32)
        trt = Tok()
        gring = Ring(S, lp + "g", [128, TB], F32, 2)
        sring = Ring(S, lp + "sg", [128, TB], F32, 2)
        lring = Ring(S, lp + "l", [128, TB], F32, 2)
        W1 = I[lp + "exp_w1"].rearrange("(e kt p) c -> e p kt c", p=128, kt=KT)
        W2 = I[lp + "exp_w2"].rearrange("(e j p) c -> e p j c", p=128, j=16)
        for b in range(B):
            for (ldf, stf, binfo) in blks(b, "moe%d" % layer):
                ldf()
                front(pfx, b)
                for s in range(4):
                    ps, ptk = S.ps()
                    for kt in range(KT):
                        S.op("pe", lambda e, kt=kt, s=s: e.matmul(ps[:, 0:NE], lhsT=hT[:, kt, s * 128:(s + 1) * 128],
                                                                  rhs=rw[:, kt, :], start=(kt == 0), stop=(kt == KT - 1)),
                             r=[thT, trw], w=[ptk])
                    S.op("dve", lambda e: e.tensor_tensor(out=lg[:], in0=ps[:, 0:NE], in1=rb[:], op=ALU.add),
                         r=[ptk, trb], w=[trt])
                    S.op("dve", lambda e: e.max(out=m8[:], in_=lg[:]), r=[trt], w=[trt])
                    S.op("dve", lambda e: e.tensor_scalar(out=msk[:], in0=lg[:], scalar1=m8[:, 3:4], scalar2=None,
                                                          op0=ALU.is_ge), r=[trt], w=[trt])
                    S.op("dve", lambda e: e.tensor_scalar(out=ex[:], in0=lg[:], scalar1=m8[:, 0:1], scalar2=None,
                                                          op0=ALU.subtract), r=[trt], w=[trt])
                    S.op("act", lambda e: e.activation(out=ex[:], in_=ex[:], func=AF.Exp), r=[trt], w=[trt])
                    S.op("dve", lambda e: e.tensor_tensor(out=ex[:], in0=ex[:], in1=msk[:], op=ALU.mult), r=[trt], w=[trt])
                    S.op("dve", lambda e: e.tensor_reduce(out=den[:], in_=ex[:], axis=AX.X, op=ALU.add), r=[trt], w=[trt])
                    S.op("dve", lambda e: e.reciprocal(out=den[:], in_=den[:]), r=[trt], w=[trt])
                    S.op("dve", lambda e: e.tensor_scalar(out=ex[:], in0=ex[:], scalar1=den[:, 0:1], scalar2=None,
                                                          op0=ALU.mult), r=[trt], w=[trt])
                    ps2, ptk2 = S.ps()
                    S.op("pe", lambda e: e.transpose(ps2[0:NE, 0:128], ex[:], ident[:]), r=[trt, tc_], w=[ptk2])
                    S.op("act", lambda e, s=s: e.activation(out=gT[:, s * 128:(s + 1) * 128], in_=ps2[0:NE, 0:128],
                                                            func=AF.Copy), r=[ptk2], w=[tgT])
                for ei in range(NE):
                    psg, ptkg = S.ps()
                    S.op("pe", lambda e, ei=ei: e.matmul(psg[:], lhsT=sel[:, ei, :], rhs=gT[:], start=True, stop=True),
                         r=[tsel, tgT], w=[ptkg])
                    S.op("act", lambda e: e.activation(out=gbc[:], in_=psg[:], func=AF.Copy), r=[ptkg], w=[tgbc])
                    for j in range(16):
                        if j % 2 == 0:
                            wt, wtk = w1ring.next()
                            S.dma("pool", wt[:], W1[ei][:, :, j * 256:(j + 2) * 256], w=[wtk])
                        jo = (j % 2) * 256
                        pg, tpg = S.ps()
                        pl, tpl = S.ps()
                        for (pp, tp, off) in ((pg, tpg, 0), (pl, tpl, 1)):
                            for kt in range(KT):
                                S.op("pe", lambda e, pp=pp, kt=kt, off=off, jo=jo, wt=wt: e.matmul(
                                    pp[:], lhsT=wt[:, kt, jo + off:jo + 256:2], rhs=hT[:, kt, :], start=(kt == 0),
                                    stop=(kt == KT - 1)), r=[wtk, thT], w=[tp])
                        g, tg = gring.next()
                        sg, tsg = sring.next()
                        ll, tl = lring.next()
                        S.op("dve", lambda e, j=j, ei=ei: e.tensor_scalar(out=g[:], in0=pg[:], scalar1=b1g[:, j, ei:ei + 1],
                                                                           scalar2=7.0, op0=ALU.add, op1=ALU.min),
                             r=[tpg, tb1], w=[tg])
                        S.op("act", lambda e: e.activation(out=sg[:], in_=g[:], func=AF.Sigmoid, scale=1.702),
                             r=[tg], w=[tsg])
                        S.op("dve", lambda e, j=j, ei=ei: e.tensor_scalar(out=ll[:], in0=pl[:], scalar1=b1l[:, j, ei:ei + 1],
                                                                           scalar2=-7.0, op0=ALU.add, op1=ALU.max),
                             r=[tpl, tb1], w=[tl])
                        S.op("pool", lambda e: e.tensor_scalar(out=ll[:], in0=ll[:], scalar1=7.0, scalar2=1.0,
                                                               op0=ALU.min, op1=ALU.add), r=[tl], w=[tl])
                        S.op("pool", lambda e: e.tensor_tensor(out=g[:], in0=g[:], in1=sg[:], op=ALU.mult),
                             r=[tg, tsg], w=[tg])
                        S.op("dve", lambda e, j=j: e.tensor_tensor(out=actT[:, j, :], in0=g[:], in1=ll[:], op=ALU.mult),
                             r=[tg, tl], w=[tact[j]])
                    for m in range(16):
                        wt, wtk = w2ring.next()
                        S.dma("pool", wt[:], W2[ei][:, :, m * 128:(m + 1) * 128], w=[wtk])
                        py, tpy = S.ps()
                        for j in range(16):
                            S.op("pe", lambda e, j=j: e.matmul(py[:], lhsT=wt[:, j, :], rhs=actT[:, j, :],
                                                               start=(j == 0), stop=(j == 15)), r=[wtk, tact[j]], w=[tpy])
                        tm, ttm = tmpring.next()
                        S.op("dve", lambda e, m=m, ei=ei: e.scalar_tensor_tensor(
                            out=tm[:], in0=py[:], scalar=b2c[:, m, ei:ei + 1], in1=gbc[:], op0=ALU.add, op1=ALU.mult),
                            r=[tpy, tb1, tgbc], w=[ttm])
                        S.op("dve", lambda e, m=m: e.scalar_tensor_tensor(
                            out=xT[:, m, :], in0=tm[:], scalar=md[:, 32 + m, b:b + 1], in1=xT[:, m, :],
                            op0=ALU.mult, op1=ALU.add), r=[ttm, tmd, txT], w=[txT])
                stf()

    def conv():
        md, Am, tmd = mods["l1_mix"]
        bp1, tbp1 = col_load("bpw1", I["l1_conv_b_pw1"], 32)
        bdw, tbdw = col_load("bdw", I["l1_conv_b_dw"], 16)
        lng, tlng = col_load("lng", I["l1_conv_ln_g"], 16)
        lnb, tlnb = col_load("lnb", I["l1_conv_ln_b"], 16)
        bp2, tbp2 = col_load("bpw2", I["l1_conv_b_pw2"], 16)
        wn = S.sbuf("wdwn", [31, D], F32)
        twn = Tok()
        S.dma("sp", wn[:], I["l1_conv_w_dw"], w=[twn])
        wdw = S.sbuf("wdw", [128, KT, 31], F32)
        twd = Tok()
        for kt in range(KT):
            ps, ptk = S.ps()
            S.op("pe", lambda e, kt=kt: e.transpose(ps[:, 0:31], wn[0:31, kt * 128:(kt + 1) * 128], ident[0:31, 0:31]),
                 r=[twn, tc_], w=[ptk])
            S.op("dve", lambda e, kt=kt: e.tensor_copy(wdw[:, kt, :], ps[:, 0:31]), r=[ptk], w=[twd])
        y1h = S.sbuf("y1h", [128, KT, 30 + TB], F32)
        ty1 = Tok()
        y2 = S.sbuf("y2", [128, KT, TB], F32)
        ty2 = Tok()
        y3T = S.sbuf("y3T", [128, KT, TB], BF16)
        ty3 = Tok()
        mean = S.sbuf("lnmean", [128, TB], F32)
        lrs = S.sbuf("lnrstd", [128, TB], F32)
        tln = Tok()
        sgr = Ring(S, "csg", [128, TB], F32, 2)
        for b in range(B):
            S.op("pool", lambda e: e.memset(y1h[:, :, 0:30], 0.0), w=[ty1])
            for (ldf, stf, binfo) in blks(b, "conv"):
                ldf()
                front("l1_mix", b)
                for m in range(16):
                    pv, tpv = lin_group(hT, thT, KT, I["l1_conv_w_pw1"], m * 128, 128, TB)
                    pg, tpg = lin_group(hT, thT, KT, I["l1_conv_w_pw1"], D + m * 128, 128, TB)
                    sg, tsg = sgr.next()
                    S.op("act", lambda e, m=m: e.activation(out=sg[:], in_=pg[:], func=AF.Sigmoid,
                                                            bias=bp1[:, 16 + m:17 + m]), r=[tpg, tbp1], w=[tsg])
                    S.op("dve", lambda e, m=m: e.scalar_tensor_tensor(
                        out=y1h[:, m, 30:30 + TB], in0=pv[:], scalar=bp1[:, m:m + 1], in1=sg[:], op0=ALU.add,
                        op1=ALU.mult), r=[tpv, tsg, tbp1], w=[ty1])
                if binfo["halo"]:
                    S.op("dve", lambda e: e.tensor_scalar(out=y1h[:, :, TB:TB + 30], in0=y1h[:, :, TB:TB + 30],
                                                          scalar1=hv_sb[:, 0:1], scalar2=None, op0=ALU.mult),
                         r=[ty1, thv], w=[ty1])
                    S.op("pool", lambda e: e.tensor_copy(y1h[:, :, 0:30], y1h[:, :, TB:TB + 30]), r=[ty1], w=[ty1])
                    continue
                for m in range(16):
                    S.op("dve", lambda e, m=m: e.tensor_scalar(out=y2[:, m, :], in0=y1h[:, m, 0:TB],
                                                               scalar1=wdw[:, m, 0:1], scalar2=bdw[:, m:m + 1],
                                                               op0=ALU.mult, op1=ALU.add), r=[ty1, twd, tbdw], w=[ty2])
                    for k in range(1, 31):
                        S.op("dve", lambda e, m=m, k=k: e.scalar_tensor_tensor(
                            out=y2[:, m, :], in0=y1h[:, m, k:k + TB], scalar=wdw[:, m, k:k + 1], in1=y2[:, m, :],
                            op0=ALU.mult, op1=ALU.add), r=[ty1, twd], w=[ty2])
                S.op("pool", lambda e: e.tensor_copy(y1h[:, :, 0:30], y1h[:, :, TB:TB + 30]), r=[ty2], w=[ty1])
                psum_, tps = S.ps()
                psq, tpq = S.ps()
                for m in range(16):
                    sq, tsq = sqring.next()
                    S.op("act", lambda e, m=m: e.activation(out=sq[:], in_=y2[:, m, :], func=AF.Square), r=[ty2], w=[tsq])
                    S.op("pe", lambda e, m=m: e.matmul(psum_[:], lhsT=ones[:], rhs=y2[:, m, :], start=(m == 0), stop=(m == 15)),
                         r=[ty2, tc_], w=[tps])
                    S.op("pe", lambda e, m=m: e.matmul(psq[:], lhsT=ones[:], rhs=sq[:], start=(m == 0), stop=(m == 15)),
                         r=[tsq, tc_], w=[tpq])
                S.op("dve", lambda e: e.tensor_scalar(out=mean[:], in0=psum_[:], scalar1=1.0 / D, scalar2=None,
                                                      op0=ALU.mult), r=[tps], w=[tln])
                S.op("dve", lambda e: e.tensor_tensor(out=lrs[:], in0=mean[:], in1=mean[:], op=ALU.mult), r=[tln], w=[tln])
                S.op("dve", lambda e: e.scalar_tensor_tensor(out=lrs[:], in0=psq[:], scalar=1.0 / D, in1=lrs[:],
                                                             op0=ALU.mult, op1=ALU.subtract), r=[tpq, tln], w=[tln])
                S.op("dve", lambda e: e.tensor_scalar(out=lrs[:], in0=lrs[:], scalar1=1e-5, scalar2=None, op0=ALU.add),
                     r=[tln], w=[tln])
                S.op("act", lambda e: e.activation(out=lrs[:], in_=lrs[:], func=AF.Sqrt), r=[tln], w=[tln])
                S.op("dve", lambda e: e.reciprocal(out=lrs[:], in_=lrs[:]), r=[tln], w=[tln])
                for m in range(16):
                    S.op("dve", lambda e, m=m: e.tensor_tensor(out=y2[:, m, :], in0=y2[:, m, :], in1=mean[:], op=ALU.subtract),
                         r=[tln], w=[ty2])
                    S.op("pool", lambda e, m=m: e.tensor_tensor(out=y2[:, m, :], in0=y2[:, m, :], in1=lrs[:], op=ALU.mult),
                         r=[tln], w=[ty2])
                    S.op("pool", lambda e, m=m: e.tensor_scalar(out=y2[:, m, :], in0=y2[:, m, :], scalar1=lng[:, m:m + 1],
                                                                scalar2=lnb[:, m:m + 1], op0=ALU.mult, op1=ALU.add),
                         r=[tlng, tlnb], w=[ty2])
                    S.op("act", lambda e, m=m: e.activation(out=y3T[:, m, :], in_=y2[:, m, :], func=AF.Silu), r=[ty2], w=[ty3])
                for m in range(16):
                    ps, ptk = lin_group(y3T, ty3, KT, I["l1_conv_w_pw2"], m * 128, 128, TB)
                    tm, ttm = tmpring.next()
                    S.op("dve", lambda e, m=m: e.tensor_scalar(out=tm[:], in0=ps[:], scalar1=bp2[:, m:m + 1],
                                                               scalar2=md[:, 32 + m, b:b + 1], op0=ALU.add, op1=ALU.mult),
                         r=[ptk, tbp2, tmd], w=[ttm])
                    S.op("pool", lambda e, m=m: e.tensor_tensor(out=xT[:, m, :], in0=xT[:, m, :], in1=tm[:], op=ALU.add),
                         r=[ttm, txT], w=[txT])
                stf()

    ymix = [nc.dram_tensor("ymix%d" % b, [128, 16, L], BF16).ap() for b in range(B)]
    tymix = [[[Tok() for _ in range(NB)] for _ in range(16)] for b in range(B)]
    qTd = [nc.dram_tensor("qT%d" % b, [128, 8, L], BF16).ap() for b in range(B)]
    qpeTd = [nc.dram_tensor("qpeT%d" % b, [64, 8, L], BF16).ap() for b in range(B)]
    kTd = [nc.dram_tensor("kT%d" % b, [128, 8, L], BF16).ap() for b in range(B)]
    kpeTd = [nc.dram_tensor("kpeT%d" % b, [64, L], BF16).ap() for b in range(B)]
    vtd = [nc.dram_tensor("vt%d" % b, [128, NT, 1024], BF16).ap() for b in range(B)]
    tqk = [[Tok() for _ in range(NB)] for b in range(B)]

    def s5_pass():
        SP = [128, 32]
        tp = Tok()

        def t32(name):
            return S.sbuf("s5_" + name, SP, F32)
        mag, cs, sn, t1, t2, ck, sk, m128 = [t32(n) for n in ("mag", "cs", "sn", "t1", "t2", "ck", "sk", "m128")]
        COS = S.sbuf("s5_COS", [128, 32, 128], F32)
        SIN = S.sbuf("s5_SIN", [128, 32, 128], F32)
        LBr = S.sbuf("s5_LBr", [128, 32, 128], BF16)
        LBi = S.sbuf("s5_LBi", [128, 32, 128], BF16)
        LCr = S.sbuf("s5_LCr", [128, 32, 128], BF16)
        LCrn = S.sbuf("s5_LCrn", [128, 32, 128], BF16)
        LCin = S.sbuf("s5_LCin", [128, 32, 128], BF16)
        dcol, tdc = col_load("s5_d", I["l0_s5_d"].rearrange("g h -> (g h)"), 8)
        bglu, tbg = col_load("s5_bglu", I["l0_s5_b_glu"], 8)
        prep_scope = S.scope()
        prep_scope.__enter__()
        lre, lim, lst = t32("lre"), t32("lim"), t32("lst")
        S.dma("sp", lre[:], I["l0_s5_lambda_re"].rearrange("g p -> (g p)").rearrange("(gp q) -> q gp", q=128), w=[tp])
        S.dma("sp", lim[:], I["l0_s5_lambda_im"].rearrange("g p -> (g p)").rearrange("(gp q) -> q gp", q=128), w=[tp])
        lsv = I["l0_s5_log_step"].rearrange("(gp two) -> two gp", two=2)
        for two in range(2):
            S.dma("sp", lst[64 * two:64 * two + 64, :], lsv[two:two + 1, :].partition_broadcast(64), w=[tp])
        halfpi = S.sbuf("s5_halfpi", [128, 1], F32)
        S.op("dve", lambda e: e.memset(halfpi[:], math.pi / 2), w=[tp])
        step, a_, th = [t32(n) for n in ("step", "a", "th")]
        dv = lambda fn: S.op("dve", fn, r=[tp], w=[tp])
        ac = lambda fn: S.op("act", fn, r=[tp], w=[tp])
        dv(lambda e: e.tensor_scalar(out=lre[:], in0=lre[:], scalar1=-1e-4, scalar2=None, op0=ALU.min))
        ac(lambda e: e.activation(out=step[:], in_=lst[:], func=AF.Exp))
        dv(lambda e: e.tensor_tensor(out=a_[:], in0=lre[:], in1=step[:], op=ALU.mult))
        dv(lambda e: e.tensor_tensor(out=th[:], in0=lim[:], in1=step[:], op=ALU.mult))
        ac(lambda e: e.activation(out=mag[:], in_=a_[:], func=AF.Exp))
        ac(lambda e: e.activation(out=sn[:], in_=th[:], func=AF.Sin, scale=1.0 / 64))
        ac(lambda e: e.activation(out=cs[:], in_=th[:], func=AF.Sin, scale=1.0 / 64, bias=halfpi[:, 0:1]))

        def dbl(c, s_):
            dv(lambda e: e.tensor_tensor(out=t1[:], in0=c[:], in1=c[:], op=ALU.mult))
            dv(lambda e: e.tensor_tensor(out=t2[:], in0=s_[:], in1=s_[:], op=ALU.mult))
            dv(lambda e: e.scalar_tensor_tensor(out=s_[:], in0=c[:], scalar=2.0, in1=s_[:], op0=ALU.mult, op1=ALU.mult))
            dv(lambda e: e.tensor_tensor(out=c[:], in0=t1[:], in1=t2[:], op=ALU.subtract))
        for _ in range(6):
            dbl(cs, sn)
        abre, abim, den, nr, rre, rim = [t32(n) for n in ("abre", "abim", "den", "nr", "rre", "rim")]
        dv(lambda e: e.tensor_tensor(out=abre[:], in0=mag[:], in1=cs[:], op=ALU.mult))
        dv(lambda e: e.tensor_tensor(out=abim[:], in0=mag[:], in1=sn[:], op=ALU.mult))
        dv(lambda e: e.tensor_tensor(out=t1[:], in0=lre[:], in1=lre[:], op=ALU.mult))
        dv(lambda e: e.tensor_tensor(out=t2[:], in0=lim[:], in1=lim[:], op=ALU.mult))
        dv(lambda e: e.tensor_tensor(out=den[:], in0=t1[:], in1=t2[:], op=ALU.add))
        dv(lambda e: e.reciprocal(out=den[:], in_=den[:]))
        dv(lambda e: e.tensor_scalar(out=nr[:], in0=abre[:], scalar1=-1.0, scalar2=None, op0=ALU.add))
        dv(lambda e: e.tensor_tensor(out=t1[:], in0=nr[:], in1=lre[:], op=ALU.mult))
        dv(lambda e: e.tensor_tensor(out=t2[:], in0=abim[:], in1=lim[:], op=ALU.mult))
        dv(lambda e: e.tensor_tensor(out=rre[:], in0=t1[:], in1=t2[:], op=ALU.add))
        dv(lambda e: e.tensor_tensor(out=rre[:], in0=rre[:], in1=den[:], op=ALU.mult))
        dv(lambda e: e.tensor_tensor(out=t1[:], in0=abim[:], in1=lre[:], op=ALU.mult))
        dv(lambda e: e.tensor_tensor(out=t2[:], in0=nr[:], in1=lim[:], op=ALU.mult))
        dv(lambda e: e.tensor_tensor(out=rim[:], in0=t1[:], in1=t2[:], op=ALU.subtract))
        dv(lambda e: e.tensor_tensor(out=rim[:], in0=rim[:], in1=den[:], op=ALU.mult))
        bnr = S.sbuf("s5_bnr", [128, 32, 16], F32)
        bni = S.sbuf("s5_bni", [128, 32, 16], F32)
        bbr = S.sbuf("s5_bbr", [128, 32, 16], F32)
        bbi = S.sbuf("s5_bbi", [128, 32, 16], F32)
        tq = S.sbuf("s5_tq", [128, 32, 16], F32)
        S.dma("sp", bnr[:], I["l0_s5_b_re"].rearrange("g p h -> (g p) h").rearrange("(gp q) h -> q gp h", q=128), w=[tp])
        S.dma("sp", bni[:], I["l0_s5_b_im"].rearrange("g p h -> (g p) h").rearrange("(gp q) h -> q gp h", q=128), w=[tp])
        bc = lambda t: t[:].unsqueeze(2).to_broadcast([128, 32, 16])
        dv(lambda e: e.tensor_tensor(out=bbr[:], in0=bnr[:], in1=bc(rre), op=ALU.mult))
        dv(lambda e: e.tensor_tensor(out=tq[:], in0=bni[:], in1=bc(rim), op=ALU.mult))
        dv(lambda e: e.tensor_tensor(out=bbr[:], in0=bbr[:], in1=tq[:], op=ALU.subtract))
        dv(lambda e: e.tensor_tensor(out=bbi[:], in0=bni[:], in1=bc(rre), op=ALU.mult))
        dv(lambda e: e.tensor_tensor(out=tq[:], in0=bnr[:], in1=bc(rim), op=ALU.mult))
        dv(lambda e: e.tensor_tensor(out=bbi[:], in0=bbi[:], in1=tq[:], op=ALU.add))
        tLB = Tok()
        xpr = Ring(S, "s5_xp", [128, 128], F32, 2)
        for gp in range(32):
            gq = gp % 4
            for (bb, LB) in ((bbr, LBr), (bbi, LBi)):
                xp, txp = xpr.next()
                S.op("pool", lambda e: e.memset(xp[:], 0.0), w=[txp])
                S.op("dve", lambda e, bb=bb, gp=gp, gq=gq: e.tensor_copy(xp[0:64, gq * 32:gq * 32 + 16], bb[0:64, gp, :]),
                     r=[tp], w=[txp])
                S.op("dve", lambda e, bb=bb, gp=gp, gq=gq: e.tensor_copy(xp[64:128, gq * 32 + 16:gq * 32 + 32], bb[64:128, gp, :]),
                     r=[tp], w=[txp])
                ps, ptk = S.ps()
                S.op("pe", lambda e: e.transpose(ps[:, 0:128], xp[:], ident[:]), r=[txp, tc_], w=[ptk])
                S.op("act", lambda e, LB=LB, gp=gp: e.activation(out=LB[:, gp, :], in_=ps[:, 0:128], func=AF.Copy),
                     r=[ptk], w=[tLB])
        cnr = S.sbuf("s5_cnr", [128, 8, 64], F32)
        cni = S.sbuf("s5_cni", [128, 8, 64], F32)
        S.dma("sp", cnr[:], I["l0_s5_c_re"].rearrange("g h p -> (g h) p").rearrange("(c r) p -> r c p", r=128), w=[tp])
        S.dma("sp", cni[:], I["l0_s5_c_im"].rearrange("g h p -> (g h) p").rearrange("(c r) p -> r c p", r=128), w=[tp])
        mk = S.sbuf("s5_mk", [128, 8], F32)
        S.op("dve", lambda e: e.tensor_reduce(out=mk[:], in_=ident[:].rearrange("p (j h) -> p j h", h=16), axis=AX.X,
                                              op=ALU.add), r=[tc_], w=[tp])
        tLC = Tok()
        for gp in range(32):
            c, gq = gp // 4, gp % 4
            for (cn, outs) in ((cnr, ((LCr, 1.0), (LCrn, -1.0))), (cni, ((LCin, -1.0),))):
                xp, txp = xpr.next()
                for two in range(2):
                    S.op("dve", lambda e, cn=cn, c=c, gq=gq, two=two: e.tensor_scalar(
                        out=xp[:, 64 * two:64 * two + 64], in0=cn[:, c, :], scalar1=mk[:, 2 * gq + two:2 * gq + two + 1],
                        scalar2=None, op0=ALU.mult), r=[tp], w=[txp])
                ps, ptk = S.ps()
                S.op("pe", lambda e: e.transpose(ps[:, 0:128], xp[:], ident[:]), r=[txp, tc_], w=[ptk])
                for (LC, sgn) in outs:
                    S.op("act", lambda e, LC=LC, gp=gp, sgn=sgn: e.activation(out=LC[:, gp, :], in_=ps[:, 0:128],
                                                                              func=AF.Copy, scale=sgn), r=[ptk], w=[tLC])
        u1 = S.sbuf("s5_u1", [128, 32, 64], F32)
        u2 = S.sbuf("s5_u2", [128, 32, 64], F32)
        dv(lambda e: e.memset(COS[:, :, 0:1], 1.0))
        dv(lambda e: e.memset(SIN[:, :, 0:1], 0.0))
        dv(lambda e: e.tensor_copy(ck[:], cs[:]))
        dv(lambda e: e.tensor_copy(sk[:], sn[:]))
        dv(lambda e: e.tensor_copy(m128[:], mag[:]))
        k = 1
        while k < 128:
            bk = lambda t, k=k: t[:].unsqueeze(2).to_broadcast([128, 32, k])
            dv(lambda e, k=k: e.tensor_tensor(out=u1[:, :, 0:k], in0=COS[:, :, 0:k], in1=bk(ck), op=ALU.mult))
            dv(lambda e, k=k: e.tensor_tensor(out=u2[:, :, 0:k], in0=SIN[:, :, 0:k], in1=bk(sk), op=ALU.mult))
            dv(lambda e, k=k: e.tensor_tensor(out=COS[:, :, k:2 * k], in0=u1[:, :, 0:k], in1=u2[:, :, 0:k], op=ALU.subtract))
            dv(lambda e, k=k: e.tensor_tensor(out=u1[:, :, 0:k], in0=SIN[:, :, 0:k], in1=bk(ck), op=ALU.mult))
            dv(lambda e, k=k: e.tensor_tensor(out=u2[:, :, 0:k], in0=COS[:, :, 0:k], in1=bk(sk), op=ALU.mult))
            dv(lambda e, k=k: e.tensor_tensor(out=SIN[:, :, k:2 * k], in0=u1[:, :, 0:k], in1=u2[:, :, 0:k], op=ALU.add))
            dbl(ck, sk)
            dv(lambda e: e.tensor_tensor(out=m128[:], in0=m128[:], in1=m128[:], op=ALU.mult))
            k *= 2
        prep_scope.__exit__(None, None, None)
        ubf = S.sbuf("s5_ubf", [128, 8, TB], BF16)
        tub = Tok()
        gTb = S.sbuf("s5_gT", [128, 8, TB], BF16)
        tgTb = Tok()
        ymb = S.sbuf("s5_ymb", [128, 8, TB], BF16)
        tymb = Tok()
        WLr, WLi, WIr, WIi = t32("WLr"), t32("WLi"), t32("WIr"), t32("WIi")
        tW = Tok()
        cring = Ring(S, "s5_c", [128, 512], F32, 4)
        mring = Ring(S, "s5_m", [128, 512], F32, 4)
        wringS = Ring(S, "s5_w", [128, 512], F32, 4)
        pring = Ring(S, "s5_p", [128, 512], BF16, 6)
        ering = Ring(S, "s5_e", [128, 128], F32, 4)
        for b in range(B):
            S.op("dve", lambda e: e.memset(WIr[:], 0.0), r=[tW], w=[tW])
            S.op("dve", lambda e: e.memset(WIi[:], 0.0), r=[tW], w=[tW])
            for blk in range(NB):
                load_x_block(b, blk)
                front("l0_mix", b)
                for m in range(8):
                    ps, ptk = lin_group(hT, thT, KT, I["l0_w_in"], m * 128, 128, TB)
                    S.op("act", lambda e, m=m: e.activation(out=ubf[:, m, :], in_=ps[:], func=AF.Copy), r=[ptk], w=[tub])
                for ch in range(4):
                    c0 = ch * 128
                    for g4 in range(8):
                        pbr, tpbr = S.ps()
                        pbi, tpbi = S.ps()
                        for j in range(4):
                            gp = g4 * 4 + j
                            S.op("pe", lambda e, gp=gp, j=j: e.matmul(pbr[:, j * 128:(j + 1) * 128], lhsT=LBr[:, gp, :],
                                                                      rhs=ubf[:, g4, c0:c0 + 128], start=True, stop=True),
                                 r=[tLB, tub], w=[tpbr])
                            S.op("pe", lambda e, gp=gp, j=j: e.matmul(pbi[:, j * 128:(j + 1) * 128], lhsT=LBi[:, gp, :],
                                                                      rhs=ubf[:, g4, c0:c0 + 128], start=True, stop=True),
                                 r=[tLB, tub], w=[tpbi])
                        cosv = COS[:, g4 * 4:g4 * 4 + 4, :]
                        sinv = SIN[:, g4 * 4:g4 * 4 + 4, :]
                        v3 = lambda t: t[:].rearrange("p (j t) -> p j t", j=4)
                        m1, tm1 = mring.next()
                        m2, tm2 = mring.next()
                        cr, tcr = cring.next()
                        ci, tci = cring.next()
                        S.op("dve", lambda e: e.tensor_tensor(out=v3(m1), in0=v3(pbr), in1=cosv, op=ALU.mult), r=[tpbr, tp], w=[tm1])
                        S.op("dve", lambda e: e.tensor_tensor(out=v3(m2), in0=v3(pbi), in1=sinv, op=ALU.mult), r=[tpbi, tp], w=[tm2])
                        S.op("pool", lambda e: e.tensor_tensor(out=cr[:], in0=m1[:], in1=m2[:], op=ALU.add), r=[tm1, tm2], w=[tcr])
                        m3, tm3 = mring.next()
                        m4, tm4 = mring.next()
                        S.op("dve", lambda e: e.tensor_tensor(out=v3(m3), in0=v3(pbi), in1=cosv, op=ALU.mult), r=[tpbi, tp], w=[tm3])
                        S.op("dve", lambda e: e.tensor_tensor(out=v3(m4), in0=v3(pbr), in1=sinv, op=ALU.mult), r=[tpbr, tp], w=[tm4])
                        S.op("pool", lambda e: e.tensor_tensor(out=ci[:], in0=m3[:], in1=m4[:], op=ALU.subtract), r=[tm3, tm4], w=[tci])
                        wr, twr = wringS.next()
                        wi, twi = wringS.next()
                        for j in range(4):
                            gp = g4 * 4 + j
                            S.op("dve", lambda e, gp=gp, j=j: e.tensor_tensor_scan(
                                out=wr[:, j * 128:(j + 1) * 128], data0=mag[:, gp:gp + 1].to_broadcast([128, 128]),
                                data1=cr[:, j * 128:(j + 1) * 128], initial=WIr[:, gp:gp + 1], op0=ALU.mult, op1=ALU.add),
                                r=[tcr, tW, tp], w=[twr])
                            S.op("dve", lambda e, gp=gp, j=j: e.tensor_tensor_scan(
                                out=wi[:, j * 128:(j + 1) * 128], data0=mag[:, gp:gp + 1].to_broadcast([128, 128]),
                                data1=ci[:, j * 128:(j + 1) * 128], initial=WIi[:, gp:gp + 1], op0=ALU.mult, op1=ALU.add),
                                r=[tci, tW, tp], w=[twi])
                        S.op("pool", lambda e: e.tensor_copy(WLr[:, g4 * 4:g4 * 4 + 4], v3(wr)[:, :, 127]), r=[twr, tW], w=[tW])
                        S.op("pool", lambda e: e.tensor_copy(WLi[:, g4 * 4:g4 * 4 + 4], v3(wi)[:, :, 127]), r=[twi, tW], w=[tW])
                        prods = []
                        for (wsrc, tws, tab, eng) in ((wr, twr, cosv, "dve"), (wr, twr, sinv, "pool"),
                                                      (wi, twi, sinv, "dve"), (wi, twi, cosv, "pool")):
                            pp, tpp = pring.next()
                            S.op(eng, lambda e, wsrc=wsrc, tab=tab, pp=pp: e.tensor_tensor(out=v3(pp), in0=v3(wsrc), in1=tab, op=ALU.mult),
                                 r=[tws, tp], w=[tpp])
                            prods.append((pp, tpp))
                        py, tpy = S.ps()
                        n = 0
                        for j in range(4):
                            gp = g4 * 4 + j
                            for (LC, (pp, tpp)) in ((LCr, prods[0]), (LCin, prods[1]), (LCrn, prods[2]), (LCin, prods[3])):
                                S.op("pe", lambda e, LC=LC, gp=gp, pp=pp, j=j, n=n: e.matmul(
                                    py[:, 0:128], lhsT=LC[:, gp, :], rhs=pp[:, j * 128:(j + 1) * 128], start=(n == 0),
                                    stop=(n == 15)), r=[tLC, tpp], w=[tpy])
                                n += 1
                        yl, tyl = ering.next()
                        x2, tx2 = ering.next()
                        S.op("dve", lambda e: e.scalar_tensor_tensor(out=yl[:], in0=ubf[:, g4, c0:c0 + 128], scalar=dcol[:, g4:g4 + 1],
                                                                     in1=py[:, 0:128], op0=ALU.mult, op1=ALU.add),
                             r=[tub, tdc, tpy], w=[tyl])
                        S.op("act", lambda e: e.activation(out=x2[:], in_=yl[:], func=AF.Square), r=[tyl], w=[tx2])
                        S.op("pool", lambda e: e.tensor_scalar(out=x2[:], in0=x2[:], scalar1=0.044715, scalar2=1.0, op0=ALU.mult,
                                                               op1=ALU.add), r=[tx2], w=[tx2])
                        S.op("pool", lambda e: e.tensor_tensor(out=x2[:], in0=x2[:], in1=yl[:], op=ALU.mult), r=[tyl], w=[tx2])
                        S.op("act", lambda e: e.activation(out=x2[:], in_=x2[:], func=AF.Sigmoid, scale=1.5957691216057308),
                             r=[tx2], w=[tx2])
                        S.op("dve", lambda e: e.tensor_tensor(out=gTb[:, g4, c0:c0 + 128], in0=yl[:], in1=x2[:], op=ALU.mult),
                             r=[tyl, tx2], w=[tgTb])
                    S.op("dve", lambda e: e.tensor_tensor(out=t1[:], in0=WLr[:], in1=ck[:], op=ALU.mult), r=[tW, tp], w=[tp])
                    S.op("dve", lambda e: e.tensor_tensor(out=t2[:], in0=WLi[:], in1=sk[:], op=ALU.mult), r=[tW, tp], w=[tp])
                    S.op("dve", lambda e: e.tensor_tensor(out=WIr[:], in0=t1[:], in1=t2[:], op=ALU.subtract), r=[tp, tW], w=[tW])
                    S.op("dve", lambda e: e.tensor_tensor(out=t1[:], in0=WLr[:], in1=sk[:], op=ALU.mult), r=[tW, tp], w=[tp])
                    S.op("dve", lambda e: e.tensor_tensor(out=t2[:], in0=WLi[:], in1=ck[:], op=ALU.mult), r=[tW, tp], w=[tp])
                    S.op("dve", lambda e: e.tensor_tensor(out=WIi[:], in0=t1[:], in1=t2[:], op=ALU.add), r=[tp, tW], w=[tW])
                for m in range(8):
                    ps, ptk = lin_group(gTb, tgTb, 8, I["l0_s5_w_glu"], m * 128, 128, TB)
                    sg, tsg = tmpring.next()
                    S.op("act", lambda e, m=m: e.activation(out=sg[:], in_=ps[:], func=AF.Sigmoid, bias=bglu[:, m:m + 1]),
                         r=[ptk, tbg], w=[tsg])
                    S.op("dve", lambda e, m=m: e.tensor_tensor(out=ymb[:, m, :], in0=gTb[:, m, :], in1=sg[:], op=ALU.mult),
                         r=[tgTb, tsg], w=[tymb])
                S.dma("sp", ymix[b][:, 0:8, blk * TB:(blk + 1) * TB], ymb[:], r=[tymb], w=[tymix[b][0][blk]])

    def mla_prep():
        md, Am, tmd = mods["l0_mix"]
        qg, tqg = col_load("qng", I["l0_mla_q_norm_g"], 4)
        kg, tkg = col_load("kvng", I["l0_mla_kv_norm_g"], 4)
        tw = Tok()
        Win = I["l0_w_in"].rearrange("(kt p) c -> p kt c", p=128)
        wukv = S.sbuf("wukv", [128, 4, 2048], BF16)
        S.dma("pool", wukv[:], I["l0_mla_w_ukv"].rearrange("(kt p) c -> p kt c", p=128), w=[tw])
        wuq = S.sbuf("wuq", [128, 4, 1536], BF16)
        S.dma("pool", wuq[:], I["l0_mla_w_uq"].rearrange("(kt p) c -> p kt c", p=128), w=[tw])
        wuqs = S.sbuf("wuqs", [128, 4, 8, 64], BF16)
        Wq4 = I["l0_mla_w_uq"].rearrange("(kt p) (h c) -> p kt h c", p=128, c=192)
        for kt in range(4):
            S.dma("pool", wuqs[:, kt, :, 0:32], Wq4[:, kt, :, 160:192], w=[tw])
            S.dma("pool", wuqs[:, kt, :, 32:64], Wq4[:, kt, :, 128:160], w=[tw])
        wkr = S.sbuf("wkr", [128, KT, 64], BF16)
        wkrs = S.sbuf("wkrs", [128, KT, 64], BF16)
        S.dma("pool", wkr[:], Win[:, :, 2048:2112], w=[tw])
        S.dma("pool", wkrs[:, :, 0:32], Win[:, :, 2080:2112], w=[tw])
        S.dma("pool", wkrs[:, :, 32:64], Win[:, :, 2048:2080], w=[tw])
        pidx = S.sbuf("pidx", [64, 1], I32)
        invf = S.sbuf("invf", [64, 1], F32)
        sgn = S.sbuf("sgn", [64, 1], F32)
        tcst = Tok()
        S.op("pool", lambda e: e.iota(pidx[0:32, :], pattern=[[0, 1]], base=0, channel_multiplier=1), w=[tcst])
        S.op("pool", lambda e: e.iota(pidx[32:64, :], pattern=[[0, 1]], base=0, channel_multiplier=1), r=[tcst], w=[tcst])
        S.op("dve", lambda e: e.tensor_copy(invf[:], pidx[:]), r=[tcst], w=[tcst])
        S.op("act", lambda e: e.activation(out=invf[:], in_=invf[:], func=AF.Exp, scale=-math.log(10000.0) / 32.0),
             r=[tcst], w=[tcst])
        S.op("dve", lambda e: e.memset(sgn[0:32, :], -1.0), r=[tcst], w=[tcst])
        S.op("dve", lambda e: e.memset(sgn[32:64, :], 1.0), r=[tcst], w=[tcst])
        cq = S.sbuf("cq", [128, 4, TB], F32)
        ckv = S.sbuf("ckv", [128, 4, TB], F32)
        tcq, tckv = Tok(), Tok()
        qn = S.sbuf("qn", [128, 4, TB], BF16)
        kvn = S.sbuf("kvn", [128, 4, TB], BF16)
        tqn, tkvn = Tok(), Tok()
        posi = S.sbuf("posi", [64, TB], I32)
        ang = S.sbuf("ang", [64, TB], F32)
        rt = S.sbuf("rt", [64, TB], F32)
        rni = S.sbuf("rni", [64, TB], I32)
        rnf = S.sbuf("rnf", [64, TB], F32)
        COS2 = S.sbuf("COS2", [64, TB], F32)
        SIN2 = S.sbuf("SIN2", [64, TB], F32)
        trp = Tok()
        qblk = S.sbuf("qblk", [128, 8, TB], BF16)
        qpeblk = S.sbuf("qpeblk", [64, 8, TB], BF16)
        kblk = S.sbuf("kblk", [128, 8, TB], BF16)
        vblk = S.sbuf("vblk", [128, 4, 1024], BF16)
        kpe = S.sbuf("kpeb", [64, TB], BF16)
        tqb, tqpb, tkb, tvb, tkpe = Tok(), Tok(), Tok(), Tok(), Tok()
        r1 = Ring(S, "rp1", [64, TB], F32, 2)
        r2 = Ring(S, "rp2", [64, TB], F32, 2)

        def rms4(src, tsrc, gcol, tg, dst, tdst):
            ps, ptk = S.ps()
            for kt in range(4):
                sq, tsq = sqring.next()
                S.op("act", lambda e, kt=kt: e.activation(out=sq[:], in_=src[:, kt, :], func=AF.Square), r=[tsrc], w=[tsq])
                S.op("pe", lambda e, kt=kt: e.matmul(ps[:], lhsT=ones[:], rhs=sq[:], start=(kt == 0), stop=(kt == 3)),
                     r=[tsq, tc_], w=[ptk])
            S.op("dve", lambda e: e.tensor_scalar(out=rstd[:], in0=ps[:], scalar1=1.0 / 512, scalar2=1e-6,
                                                  op0=ALU.mult, op1=ALU.add), r=[ptk], w=[trstd])
            S.op("act", lambda e: e.activation(out=rstd[:], in_=rstd[:], func=AF.Sqrt), r=[trstd], w=[trstd])
            S.op("dve", lambda e: e.reciprocal(out=rstd[:], in_=rstd[:]), r=[trstd], w=[trstd])
            for kt in range(4):
                S.op("dve", lambda e, kt=kt: e.scalar_tensor_tensor(out=dst[:, kt, :], in0=src[:, kt, :], scalar=gcol[:, kt:kt + 1],
                                                                    in1=rstd[:], op0=ALU.mult, op1=ALU.mult),
                     r=[tsrc, tg, trstd], w=[tdst])

        def mm(ps, ptk, rows, lhs_fn, rhsT, trhs, nk, ncol=TB, r0=0):
            for kt in range(nk):
                S.op("pe", lambda e, kt=kt: e.matmul(ps[r0:r0 + rows, 0:ncol], lhsT=lhs_fn(kt), rhs=rhsT[:, kt, 0:ncol],
                                                     start=(kt == 0), stop=(kt == nk - 1)), r=[tw, trhs], w=[ptk])

        def rope_out(pa, tpa, pb, tpb, dst, tdst):
            a, ta = r1.next()
            bb, tb_ = r2.next()
            S.op("dve", lambda e: e.tensor_tensor(out=a[:], in0=pa[0:64, :], in1=COS2[:], op=ALU.mult), r=[tpa, trp], w=[ta])
            S.op("dve", lambda e: e.tensor_tensor(out=bb[:], in0=pb[0:64, :], in1=SIN2[:], op=ALU.mult), r=[tpb, trp], w=[tb_])
            S.op("pool", lambda e: e.tensor_tensor(out=dst, in0=a[:], in1=bb[:], op=ALU.add), r=[ta, tb_], w=[tdst])

        for b in range(B):
            for blk in range(NB):
                cols = slice(blk * TB, (blk + 1) * TB)
                load_x_block(b, blk)
                front("l0_mix", b)
                for m in range(4):
                    ps, ptk = lin_group(hT, thT, KT, I["l0_w_in"], 1024 + m * 128, 128, TB)
                    S.op("act", lambda e, m=m: e.activation(out=cq[:, m, :], in_=ps[:], func=AF.Copy), r=[ptk], w=[tcq])
                    ps, ptk = lin_group(hT, thT, KT, I["l0_w_in"], 1536 + m * 128, 128, TB)
                    S.op("dve", lambda e, m=m: e.tensor_copy(ckv[:, m, :], ps[:]), r=[ptk], w=[tckv])
                rms4(cq, tcq, qg, tqg, qn, tqn)
                rms4(ckv, tckv, kg, tkg, kvn, tkvn)
                S.dma("sp", posi[:], I["positions"][b:b + 1, cols].partition_broadcast(64), w=[trp])
                S.op("dve", lambda e: e.tensor_copy(ang[:], posi[:]), r=[trp], w=[trp])
                S.op("dve", lambda e: e.tensor_scalar(out=ang[:], in0=ang[:], scalar1=invf[:, 0:1], scalar2=1.0 / (2 * math.pi),
                                                      op0=ALU.mult, op1=ALU.mult), r=[trp, tcst], w=[trp])
                for (tab, off) in ((SIN2, 0.0), (COS2, 0.25)):
                    S.op("dve", lambda e, off=off: e.tensor_scalar(out=rt[:], in0=ang[:], scalar1=off, scalar2=None, op0=ALU.add),
                         r=[trp], w=[trp])
                    S.op("dve", lambda e: e.tensor_copy(rni[:], rt[:]), r=[trp], w=[trp])
                    S.op("dve", lambda e: e.tensor_copy(rnf[:], rni[:]), r=[trp], w=[trp])
                    S.op("dve", lambda e: e.tensor_tensor(out=rt[:], in0=rt[:], in1=rnf[:], op=ALU.subtract), r=[trp], w=[trp])
                    S.op("dve", lambda e: e.tensor_scalar(out=rnf[:], in0=rt[:], scalar1=0.5, scalar2=None, op0=ALU.is_gt),
                         r=[trp], w=[trp])
                    S.op("dve", lambda e: e.tensor_tensor(out=rt[:], in0=rt[:], in1=rnf[:], op=ALU.subtract), r=[trp], w=[trp])
                    S.op("dve", lambda e: e.tensor_scalar(out=rnf[:], in0=rt[:], scalar1=-0.5, scalar2=None, op0=ALU.is_lt),
                         r=[trp], w=[trp])
                    S.op("dve", lambda e: e.tensor_tensor(out=rt[:], in0=rt[:], in1=rnf[:], op=ALU.add), r=[trp], w=[trp])
                    S.op("act", lambda e, tab=tab: e.activation(out=tab[:], in_=rt[:], func=AF.Sin, scale=2 * math.pi),
                         r=[trp], w=[trp])
                S.op("dve", lambda e: e.tensor_scalar(out=SIN2[:], in0=SIN2[:], scalar1=sgn[:, 0:1], scalar2=None, op0=ALU.mult),
                     r=[trp, tcst], w=[trp])
                pa, tpa = S.ps()
                pb, tpb = S.ps()
                mm(pa, tpa, 64, lambda kt: wkr[:, kt, :], hT, thT, KT)
                mm(pb, tpb, 64, lambda kt: wkrs[:, kt, :], hT, thT, KT)
                rope_out(pa, tpa, pb, tpb, kpe[:], tkpe)
                S.dma("sp", kpeTd[b][:, cols], kpe[:], r=[tkpe], w=[tqk[b][blk]])
                for h in range(8):
                    ps, ptk = S.ps()
                    mm(ps, ptk, 128, lambda kt, h=h: wuq[:, kt, h * 192:h * 192 + 128], qn, tqn, 4)
                    S.op("act", lambda e, h=h: e.activation(out=qblk[:, h, :], in_=ps[:], func=AF.Copy), r=[ptk], w=[tqb])
                    pa, tpa = S.ps()
                    pb, tpb = S.ps()
                    mm(pa, tpa, 64, lambda kt, h=h: wuq[:, kt, h * 192 + 128:h * 192 + 192], qn, tqn, 4)
                    mm(pb, tpb, 64, lambda kt, h=h: wuqs[:, kt, h, :], qn, tqn, 4)
                    rope_out(pa, tpa, pb, tpb, qpeblk[:, h, :], tqpb)
                    ps, ptk = S.ps()
                    mm(ps, ptk, 128, lambda kt, h=h: wukv[:, kt, h * 256:h * 256 + 128], kvn, tkvn, 4)
                    S.op("dve", lambda e, h=h: e.tensor_copy(kblk[:, h, :], ps[:]), r=[ptk], w=[tkb])
                S.dma("sp", qTd[b][:, :, cols], qblk[:], r=[tqb], w=[tqk[b][blk]])
                S.dma("sp", qpeTd[b][:, :, cols], qpeblk[:], r=[tqpb], w=[tqk[b][blk]])
                S.dma("sp", kTd[b][:, :, cols], kblk[:], r=[tkb], w=[tqk[b][blk]])
                wv = wukv[:].rearrange("p kt (h c) -> p kt h c", c=256)
                for s_ in range(4):
                    for half in range(2):
                        ps, ptk = S.ps()
                        for kt in range(4):
                            S.op("pe", lambda e, kt=kt, s_=s_, half=half: e.matmul(
                                ps[:].rearrange("p (h c) -> p h c", c=128), lhsT=kvn[:, kt, s_ * 128:(s_ + 1) * 128],
                                rhs=wv[:, kt, half * 4:half * 4 + 4, 128:256], start=(kt == 0), stop=(kt == 3)),
                                r=[tw, tkvn], w=[ptk])
                        S.op("act" if half else "dve", lambda e, s_=s_, half=half: (
                            e.activation(out=vblk[:, s_, half * 512:(half + 1) * 512], in_=ps[:], func=AF.Copy) if half else
                            e.tensor_copy(vblk[:, s_, half * 512:(half + 1) * 512], ps[:])), r=[ptk], w=[tvb])
                S.dma("sp", vtd[b][:, blk * 4:(blk + 1) * 4, :], vblk[:], r=[tvb], w=[tqk[b][blk]])

    def mla_attn():
        scale = 1.0 / math.sqrt(192.0)
        onesT = S.sbuf("onesT", [128, TB], F32)
        mask = S.sbuf("cmask", [128, 4, TB], F32)
        onesb = S.sbuf("onesb", [128, 128], BF16)
        tmk = Tok()
        S.op("pool", lambda e: e.memset(onesT[:], 1.0), w=[tmk])
        S.op("pool", lambda e: e.memset(onesb[:], 1.0), w=[tmk])
        for d in range(4):
            S.op("pool", lambda e, d=d: e.affine_select(out=mask[:, d, :], in_=onesT[:], pattern=[[1, TB]],
                                                        compare_op=ALU.is_ge, fill=0.0, base=-128 * d,
                                                        channel_multiplier=-1), r=[tmk], w=[tmk])
        Kh = S.sbuf("Kh", [128, L], BF16)
        Kpe = S.sbuf("Kpe", [64, L], BF16)
        Vh = S.sbuf("Vh", [128, NT, 128], BF16)
        Qh = S.sbuf("Qh", [128, L], BF16)
        Qpe = S.sbuf("Qpe", [64, L], BF16)
        tK, tKpe, tV, tQ, tQpe = Tok(), Tok(), Tok(), Tok(), Tok()
        pring = Ring(S, "attp", [128, TB], BF16, 4)
        ering = Ring(S, "atte", [128, TB], F32, 2)
        oring = Ring(S, "atto", [128, TB], BF16, 2)
        rdr = Ring(S, "attrd", [128, TB], F32, 2)
        S.nrot = 6
        po, tpo = S.pst[6]
        pd, tpd = S.pst[7]
        for b in range(B):
            allq = [tqk[b][blk] for blk in range(NB)]
            S.dma("sp", Kpe[:], kpeTd[b][:, :], r=allq, w=[tKpe])
            for h in range(8):
                S.dma("sp", Kh[:], kTd[b][:, h, :], r=allq, w=[tK])
                S.dma("act", Vh[:], vtd[b][:, :, h * 128:(h + 1) * 128], r=allq, w=[tV])
                S.dma("sp", Qh[:], qTd[b][:, h, :], r=allq, w=[tQ])
                S.dma("act", Qpe[:], qpeTd[b][:, h, :], r=allq, w=[tQpe])
                for qb in range(NB):
                    qc = slice(qb * TB, (qb + 1) * TB)
                    nkt = 4 * (qb + 1)
                    for kt in range(nkt):
                        kc = slice(kt * 128, (kt + 1) * 128)
                        ps, ptk = S.ps()
                        S.op("pe", lambda e: e.matmul(ps[:], lhsT=Kh[:, kc], rhs=Qh[:, qc], start=True, stop=False),
                             r=[tK, tQ], w=[ptk])
                        S.op("pe", lambda e: e.matmul(ps[:], lhsT=Kpe[:, kc], rhs=Qpe[:, qc], start=False, stop=True),
                             r=[tKpe, tQpe], w=[ptk])
                        pT, tpT = pring.next()
                        d = kt - 4 * qb
                        if d < 0:
                            S.op("act", lambda e: e.activation(out=pT[:], in_=ps[:], func=AF.Exp, scale=scale), r=[ptk], w=[tpT])
                        else:
                            ef, tef = ering.next()
                            S.op("act", lambda e: e.activation(out=ef[:], in_=ps[:], func=AF.Exp, scale=scale), r=[ptk], w=[tef])
                            S.op("pool", lambda e: e.tensor_tensor(out=pT[:], in0=ef[:], in1=mask[:, d, :], op=ALU.mult),
                                 r=[tef, tmk], w=[tpT])
                        S.op("pe", lambda e: e.matmul(po[:], lhsT=Vh[:, kt, :], rhs=pT[:], start=(kt == 0), stop=(kt == nkt - 1)),
                             r=[tV, tpT], w=[tpo])
                        S.op("pe", lambda e: e.matmul(pd[:], lhsT=onesb[:], rhs=pT[:], start=(kt == 0), stop=(kt == nkt - 1)),
                             r=[tmk, tpT], w=[tpd])
                    rd, trd = rdr.next()
                    ob, tob = oring.next()
                    S.op("dve", lambda e: e.reciprocal(out=rd[:], in_=pd[:]), r=[tpd], w=[trd])
                    S.op("dve", lambda e: e.tensor_tensor(out=ob[:], in0=po[:], in1=rd[:], op=ALU.mult), r=[tpo, trd], w=[tob])
                    S.dma("sp", ymix[b][:, 8 + h, qc], ob[:], r=[tob], w=[tymix[b][8 + h][qb]])
        S.nrot = 8

    def wout_pass():
        md, Am, tmd = mods["l0_mix"]
        ymb = S.sbuf("wo_ymb", [128, 16, TB], BF16)
        tymb = Tok()
        for b in range(B):
            for blk in range(NB):
                S.dma("sp", ymb[:], ymix[b][:, :, blk * TB:(blk + 1) * TB],
                      r=[tymix[b][0][blk]] + [tymix[b][8 + h][blk] for h in range(8)], w=[tymb])
                load_x_block(b, blk)
                for m in range(16):
                    ps, ptk = lin_group(ymb, tymb, KT, I["l0_w_out"], m * 128, 128, TB)
                    S.op("dve", lambda e, m=m: e.scalar_tensor_tensor(out=xT[:, m, :], in0=ps[:], scalar=md[:, 32 + m, b:b + 1],
                                                                      in1=xT[:, m, :], op0=ALU.mult, op1=ALU.add),
                         r=[ptk, tmd, txT], w=[txT])
                store_x_block(b, blk)

    def final():
        fg, tfg = col_load("fng", I["final_norm_g"], 16)
        oring = Ring(S, "ot", [128, D], F32, 2)
        for b in range(B):
            for (ldf, stf, binfo) in blks(b, "final"):
                ldf()
                ps, ptk = S.ps()
                for kt in range(KT):
                    sq, tsq = sqring.next()
                    S.op("act", lambda e, kt=kt: e.activation(out=sq[:], in_=xT[:, kt, :], func=AF.Square), r=[txT], w=[tsq])
                    S.op("pe", lambda e, kt=kt: e.matmul(ps[:], lhsT=ones[:], rhs=sq[:], start=(kt == 0), stop=(kt == KT - 1)),
                         r=[tsq, tc_], w=[ptk])
                S.op("dve", lambda e: e.tensor_scalar(out=rstd[:], in0=ps[:], scalar1=1.0 / D, scalar2=1e-6,
                                                      op0=ALU.mult, op1=ALU.add), r=[ptk], w=[trstd])
                S.op("act", lambda e: e.activation(out=rstd[:], in_=rstd[:], func=AF.Sqrt), r=[trstd], w=[trstd])
                S.op("dve", lambda e: e.reciprocal(out=rstd[:], in_=rstd[:]), r=[trstd], w=[trstd])
                for kt in range(KT):
                    S.op("dve", lambda e, kt=kt: e.scalar_tensor_tensor(
                        out=xT[:, kt, :], in0=xT[:, kt, :], scalar=fg[:, kt:kt + 1], in1=rstd[:], op0=ALU.mult,
                        op1=ALU.mult), r=[txT, tfg, trstd], w=[txT])
                for s in range(4):
                    ot, tot = oring.next()
                    for g in range(4):
                        ps, ptk = S.ps()
                        for j in range(4):
                            kt = g * 4 + j
                            S.op("pe", lambda e, kt=kt, j=j, s=s: e.transpose(
                                ps[:, j * 128:(j + 1) * 128], xT[:, kt, s * 128:(s + 1) * 128], ident[:]),
                                r=[txT, tc_], w=[ptk])
                        S.op("act" if g % 2 else "dve", lambda e, g=g: (e.activation(
                            out=ot[:, g * 512:(g + 1) * 512], in_=ps[:], func=AF.Copy) if g % 2 else e.tensor_copy(
                            ot[:, g * 512:(g + 1) * 512], ps[:])), r=[ptk], w=[tot])
                    r0 = binfo["orow"] + s * 128
                    S.dma("sp", out[r0:r0 + 128, :], ot[:], r=[tot], w=[tout])

    stages = cfg.get("stages", ("mix0", "moe0", "conv", "moe1"))
    with S.scope():
        stage0()
    if any(k in stages for k in ("mix0", "s5", "mla")):
        def zero_half(lo):
            with S.scope():
                zt = S.sbuf("zt", [128, 8, TB], BF16)
                tz = Tok()
                S.op("dve", lambda e: e.memset(zt[:], 0.0), w=[tz])
                for b in range(B):
                    for blk in range(NB):
                        if lo == 0:
                            S.dma("sp", ymix[b][:, 0:8, blk * TB:(blk + 1) * TB], zt[:], r=[tz], w=[tymix[b][0][blk]])
                        else:
                            for h in range(8):
                                S.dma("sp", ymix[b][:, 8 + h, blk * TB:(blk + 1) * TB], zt[:, h, :], r=[tz],
                                      w=[tymix[b][8 + h][blk]])
        if "mix0" in stages or "s5" in stages:
            with S.scope():
                s5_pass()
        else:
            zero_half(0)
        if "mix0" in stages or "mla" in stages:
            with S.scope():
                mla_prep()
            with S.scope():
                mla_attn()
        else:
            zero_half(8)
        with S.scope():
            wout_pass()
    if "moe0" in stages:
        with S.scope():
            moe(0)
    if "conv" in stages:
        with S.scope():
            conv()
    if "moe1" in stages:
        with S.scope():
            moe(1)
    with S.scope():
        final()
    S.finish([tout], eng="sp")
    print("ops", S.nops, flush=True)
    S.es.close()
    return nc


def _core_maps(inputs, BT, L, NE, nown):
    NQ = L // (nown * TB)
    shared = {}
    for k, v in inputs.items():
        if k in ("x", "c", "positions"):
            continue
        v = np.ascontiguousarray(v)
        if k.endswith("exp_w1"):
            v = v.reshape(NE * D, 2 * D)
        elif k.endswith("exp_w2"):
            v = v.reshape(NE * D, D)
        shared[k] = v
    maps = []
    for b in range(BT):
        xb = np.ascontiguousarray(inputs["x"][b]).reshape(L, D)
        cb = np.ascontiguousarray(inputs["c"][b:b + 1])
        pb = np.ascontiguousarray(inputs["positions"][b:b + 1])
        for q in range(NQ):
            m = dict(shared)
            m["x"], m["c"], m["positions"] = xb, cb, pb
            blk0 = q * nown
            nb = L // TB
            sel = np.zeros((nown + 1, nb), dtype=np.float32)
            sel[0, max(blk0 - 1, 0)] = 1.0
            for i in range(nown):
                sel[i + 1, blk0 + i] = 1.0
            m["selF"] = np.ascontiguousarray(np.broadcast_to(sel.reshape(1, -1), (128, (nown + 1) * nb)))
            m["hv"] = np.full((128, 1), 0.0 if q == 0 else 1.0, dtype=np.float32)
            maps.append(m)
    return maps, NQ


def kernel(**inputs):
    cfg = inputs.pop("_cfg", None)
    if cfg is not None and not cfg.get("nown"):
        B, L, NE = cfg["B"], cfg["L"], cfg["NE"]
        nc = build(cfg)
        m = {}
        for k, v in inputs.items():
            v = np.ascontiguousarray(v)
            if k == "x":
                v = v.reshape(B * L, D)
            elif k.endswith("exp_w1"):
                v = v.reshape(NE * D, 2 * D)
            elif k.endswith("exp_w2"):
                v = v.reshape(NE * D, D)
            m[k] = v
        res = run_bass_kernel_spmd(nc, [m], core_ids=[0])
        return res.results[0]["out"].reshape(B, L, D)
    if cfg is None:
        BT, L, NE, nown = 2, 8192, NE_FULL, 4
    else:
        BT, L, NE, nown = cfg["B"], cfg["L"], cfg["NE"], cfg["nown"]
    stages = (cfg or {}).get("stages", ("mix0", "moe0", "conv", "moe1"))
    nc = build({"B": 1, "L": L, "NE": NE, "nown": nown, "stages": stages})
    maps, NQ = _core_maps(inputs, BT, L, NE, nown)
    res = run_bass_kernel_spmd(nc, maps, core_ids=list(range(len(maps))))
    outs = [r["out"].reshape(nown * TB, D) for r in res.results]
    return np.stack([np.concatenate(outs[b * NQ:(b + 1) * NQ], axis=0) for b in range(BT)], axis=0)
```
